# Optimizing a Trainium2 kernel written in Bass

```python
import jax, jax.numpy as jnp
from jax import lax
import numpy as np

D_MODEL = 1024
BATCH = 8
SEQ = 2048
DEPTH = 1
DEC_BATCH = 128
DEC_SEQ = 4
PAST_LEN = 8192
PAGE_SIZE = 128

MLA_HEADS = 8
MLA_NOPE = 64
MLA_ROPE = 32
MLA_V = 64
MLA_KV_LORA = 256
MLA_Q_LORA = 384
MLA_SCALE = (MLA_NOPE + MLA_ROPE) ** -0.5
ROPE_BASE = 10000.0
ML_HEADS = 4
ML_DH = 128
ML_CHUNK = 64
MLA_WIDTH = MLA_HEADS * MLA_V
ML_WIDTH = ML_HEADS * ML_DH
D_MIX = MLA_WIDTH + ML_WIDTH
N_MEM = 256
MEM_HEADS = 4
MEM_HD = D_MODEL // MEM_HEADS
PEER_HEADS = 8
PEER_NKEYS = 128
PEER_N = PEER_NKEYS * PEER_NKEYS
PEER_DKEY = 128
PEER_TOPK = 16
PEER_BLOCK = 256
ATTN_BLOCK = 128
LN_EPS = 1e-5
RMS_EPS = 1e-6
ALPHA = (2 * DEPTH) ** 0.25
BETA = (8 * DEPTH) ** -0.25
IN_SIZES = (MLA_Q_LORA, MLA_KV_LORA, MLA_ROPE, ML_WIDTH, ML_WIDTH, ML_WIDTH, ML_HEADS, ML_HEADS, ML_WIDTH)
IN_TOTAL = sum(IN_SIZES)
IN_SPLITS = [int(s) for s in np.cumsum(IN_SIZES)[:-1]]

kernel_name = 'hybrid_mla_mlstm_peer_step'


def layer_norm(x, g, b):
    xf = x.astype(jnp.float32)
    mu = xf.mean(-1, keepdims=True)
    var = jnp.mean(jnp.square(xf - mu), -1, keepdims=True)
    return ((xf - mu) * lax.rsqrt(var + LN_EPS) * g + b).astype(x.dtype)


def rms_norm(x, g):
    xf = x.astype(jnp.float32)
    return (xf * lax.rsqrt(jnp.mean(xf * xf, -1, keepdims=True) + RMS_EPS) * g).astype(x.dtype)


def rope_angles(pos):
    inv = 1.0 / (ROPE_BASE ** (jnp.arange(0, MLA_ROPE, 2, dtype=jnp.float32) / MLA_ROPE))
    ang = pos.astype(jnp.float32)[:, None] * inv[None, :]
    return jnp.cos(ang), jnp.sin(ang)


def apply_rope(x, cos, sin):
    half = MLA_ROPE // 2
    x1, x2 = x[..., :half], x[..., half:]
    c = cos.astype(x.dtype)
    s = sin.astype(x.dtype)
    return jnp.concatenate([x1 * c - x2 * s, x1 * s + x2 * c], -1)


def mixer_projections(x, pos, lw):
    z = jnp.einsum('bsd,de->bse', x, lw['w_in'])
    c_q, c_kv, k_r, mq, mk, mv, i_pre, f_pre, o_pre = jnp.split(z, IN_SPLITS, axis=-1)
    cos, sin = rope_angles(pos)
    q = jnp.einsum('bsc,chd->bshd', rms_norm(c_q, lw['g_q']), lw['w_uq'])
    q_rope = apply_rope(q[..., MLA_NOPE:], cos[:, None], sin[:, None])
    q_lat = jnp.einsum('bshn,chn->bshc', q[..., :MLA_NOPE], lw['w_uk'])
    kv_lat = rms_norm(c_kv, lw['g_kv'])
    k_rope = apply_rope(k_r, cos, sin)
    B, S = x.shape[:2]
    heads = lambda t: t.reshape(B, S, ML_HEADS, ML_DH).transpose(0, 2, 1, 3).astype(jnp.float32)
    ml_q = heads(mq)
    ml_k = heads(mk) * (ML_DH ** -0.5)
    ml_v = heads(mv)
    ig = (i_pre + lw['b_i']).astype(jnp.float32).transpose(0, 2, 1)
    lf = jax.nn.log_sigmoid((f_pre + lw['b_f']).astype(jnp.float32)).transpose(0, 2, 1)
    o_gate = jax.nn.sigmoid(o_pre)
    return (q_lat, q_rope, kv_lat, k_rope), (ml_q, ml_k, ml_v, ig, lf), o_gate


def mla_attend_prompt(q_lat, q_rope, kv_lat, k_rope, w_uv):
    B, S = q_lat.shape[:2]
    nb = S // ATTN_BLOCK
    qlb = q_lat.reshape(B, nb, ATTN_BLOCK, MLA_HEADS, MLA_KV_LORA).swapaxes(0, 1)
    qrb = q_rope.reshape(B, nb, ATTN_BLOCK, MLA_HEADS, MLA_ROPE).swapaxes(0, 1)
    kpos = jnp.arange(S)

    def block(args):
        ql, qr, start = args
        s = (jnp.einsum('bqhc,bkc->bhqk', ql, kv_lat) + jnp.einsum('bqhr,bkr->bhqk', qr, k_rope)).astype(jnp.float32) * MLA_SCALE
        qpos = start + jnp.arange(ATTN_BLOCK)
        s = jnp.where(kpos[None, :] <= qpos[:, None], s, -jnp.inf)
        p = jax.nn.softmax(s, axis=-1).astype(kv_lat.dtype)
        return jnp.einsum('bhqk,bkc->bqhc', p, kv_lat)

    o = lax.map(block, (qlb, qrb, jnp.arange(nb) * ATTN_BLOCK))
    o = o.swapaxes(0, 1).reshape(B, S, MLA_HEADS, MLA_KV_LORA)
    return jnp.einsum('bshc,chv->bshv', o, w_uv).reshape(B, S, MLA_WIDTH)


def mla_attend_sample(q_lat, q_rope, kv_lat, k_rope, pool_lat, pool_rope, page_table, w_uv):
    B, T = q_lat.shape[:2]
    past_lat = pool_lat[page_table].reshape(B, -1, MLA_KV_LORA)
    past_rope = pool_rope[page_table].reshape(B, -1, MLA_ROPE)
    s_past = jnp.einsum('bqhc,bkc->bhqk', q_lat, past_lat) + jnp.einsum('bqhr,bkr->bhqk', q_rope, past_rope)
    s_new = jnp.einsum('bqhc,bkc->bhqk', q_lat, kv_lat) + jnp.einsum('bqhr,bkr->bhqk', q_rope, k_rope)
    causal = jnp.arange(T)[None, :] <= jnp.arange(T)[:, None]
    s_new = jnp.where(causal, s_new.astype(jnp.float32), -jnp.inf)
    s = jnp.concatenate([s_past.astype(jnp.float32), s_new], -1) * MLA_SCALE
    p = jax.nn.softmax(s, axis=-1).astype(kv_lat.dtype)
    P = past_lat.shape[1]
    o = jnp.einsum('bhqk,bkc->bqhc', p[..., :P], past_lat) + jnp.einsum('bhqk,bkc->bqhc', p[..., P:], kv_lat)
    return jnp.einsum('bqhc,chv->bqhv', o, w_uv).reshape(B, T, MLA_WIDTH)


def mlstm_chunk(carry, inp):
    C, n, m = carry
    q, k, v, ig, lf = inp
    L = q.shape[2]
    b = jnp.cumsum(lf, axis=-1)
    causal = jnp.arange(L)[None, :] <= jnp.arange(L)[:, None]
    D = jnp.where(causal, b[..., :, None] - b[..., None, :] + ig[..., None, :], -jnp.inf)
    inter = b + m[..., None]
    m_t = jnp.maximum(inter, D.max(-1))
    A = jnp.exp(D - m_t[..., None]) * jnp.einsum('bhtd,bhsd->bhts', q, k)
    w_inter = jnp.exp(inter - m_t)
    num = w_inter[..., None] * jnp.einsum('bhtd,bhde->bhte', q, C) + jnp.einsum('bhts,bhse->bhte', A, v)
    den = w_inter * jnp.einsum('bhtd,bhd->bht', q, n) + A.sum(-1)
    h = num / jnp.maximum(jnp.abs(den), jnp.exp(-m_t))[..., None]
    b_end = b[..., -1]
    dec = b_end[..., None] - b + ig
    m_new = jnp.maximum(b_end + m, dec.max(-1))
    a_prev = jnp.exp(b_end + m - m_new)
    w_row = jnp.exp(dec - m_new[..., None])
    C_new = a_prev[..., None, None] * C + jnp.einsum('bhs,bhsd,bhse->bhde', w_row, k, v)
    n_new = a_prev[..., None] * n + jnp.einsum('bhs,bhsd->bhd', w_row, k)
    return (C_new, n_new, m_new), h


def mlstm_prompt(q, k, v, ig, lf):
    B, H, S, d = q.shape
    nc = S // ML_CHUNK
    ch = lambda t: jnp.moveaxis(t.reshape((B, H, nc, ML_CHUNK) + t.shape[3:]), 2, 0)
    init = (jnp.zeros((B, H, d, d), jnp.float32), jnp.zeros((B, H, d), jnp.float32), jnp.zeros((B, H), jnp.float32))
    state, h = lax.scan(mlstm_chunk, init, (ch(q), ch(k), ch(v), ch(ig), ch(lf)))
    h = jnp.moveaxis(h, 0, 2).reshape(B, H, S, d)
    return h, state


def mem_kv(mem, w_mk, w_mv):
    return jnp.einsum('bmd,dhe->bmhe', mem, w_mk), jnp.einsum('bmd,dhe->bmhe', mem, w_mv)


def mem_attend(x, mem_k, mem_v, w_mq, w_mo):
    q = jnp.einsum('bsd,dhe->bshe', x, w_mq)
    s = jnp.einsum('bshe,bmhe->bhsm', q, mem_k).astype(jnp.float32) * (MEM_HD ** -0.5)
    p = jax.nn.softmax(s, axis=-1).astype(x.dtype)
    o = jnp.einsum('bhsm,bmhe->bshe', p, mem_v)
    return jnp.einsum('bshe,hed->bsd', o, w_mo)


def peer(x, w_pq, sub_k1, sub_k2, peer_u, peer_v):
    shp = x.shape
    xt = x.reshape(-1, D_MODEL)
    T = xt.shape[0]
    nb = -(-T // PEER_BLOCK)
    xt = jnp.pad(xt, ((0, nb * PEER_BLOCK - T), (0, 0)))
    half = PEER_DKEY // 2

    def block(xb):
        q = jnp.einsum('td,dhk->thk', xb, w_pq)
        s1 = jnp.einsum('thk,nk->thn', q[..., :half], sub_k1).astype(jnp.float32)
        s2 = jnp.einsum('thk,nk->thn', q[..., half:], sub_k2).astype(jnp.float32)
        v1, i1 = lax.top_k(s1, PEER_TOPK)
        v2, i2 = lax.top_k(s2, PEER_TOPK)
        cand = (v1[..., :, None] + v2[..., None, :]).reshape(xb.shape[0], PEER_HEADS, PEER_TOPK * PEER_TOPK)
        cidx = (i1[..., :, None] * PEER_NKEYS + i2[..., None, :]).reshape(xb.shape[0], PEER_HEADS, PEER_TOPK * PEER_TOPK)
        sc, j = lax.top_k(cand, PEER_TOPK)
        e = jnp.take_along_axis(cidx, j, axis=-1)
        g = jax.nn.softmax(sc, axis=-1).astype(xb.dtype)
        a = g * jax.nn.gelu(jnp.einsum('td,thkd->thk', xb, peer_u[e]), approximate=False)
        return jnp.einsum('thk,thkd->td', a, peer_v[e])

    y = lax.map(block, xt.reshape(nb, PEER_BLOCK, D_MODEL))
    return y.reshape(-1, D_MODEL)[:T].reshape(shp)


def finish_layer(x, mla_o, ml_h, o_gate, mem_k, mem_v, lw):
    B, S = x.shape[:2]
    ml_o = o_gate * ml_h.transpose(0, 2, 1, 3).reshape(B, S, ML_WIDTH).astype(x.dtype)
    mix = jnp.einsum('bse,ed->bsd', jnp.concatenate([mla_o, ml_o], -1), lw['w_out'])
    x = layer_norm(ALPHA * x + mix, lw['ln1_g'], lw['ln1_b'])
    x = layer_norm(ALPHA * x + mem_attend(x, mem_k, mem_v, lw['w_mq'], lw['w_mo']), lw['ln2_g'], lw['ln2_b'])
    y = peer(x, lw['w_pq'], lw['sub_k1'], lw['sub_k2'], lw['peer_u'], lw['peer_v'])
    return layer_norm(ALPHA * x + y, lw['ln3_g'], lw['ln3_b'])


def setup_inputs(seed: int = 0) -> dict:
    key = jax.random.key(seed)
    ks = iter(jax.random.split(key, 64))
    nrm = lambda shape, scale: jax.random.normal(next(ks), shape, jnp.float32) * scale
    gain = lambda shape: 1.0 + nrm(shape, 0.01)
    L = DEPTH
    n_pages = PAST_LEN // PAGE_SIZE
    n_used = DEC_BATCH * n_pages
    n_pool = n_used + max(1, n_used // 4)
    page_table = jax.random.permutation(next(ks), n_pool)[:n_used].reshape(DEC_BATCH, n_pages).astype(jnp.int32)
    col_scale = jnp.concatenate([
        jnp.ones((MLA_Q_LORA + MLA_KV_LORA + MLA_ROPE + 2 * ML_WIDTH,), jnp.float32),
        jnp.full((ML_WIDTH,), BETA, jnp.float32),
        jnp.ones((2 * ML_HEADS + ML_WIDTH,), jnp.float32)])
    return {
        'x_prompt': nrm((BATCH, SEQ, D_MODEL), 1.0),
        'x_sample': nrm((DEC_BATCH, DEC_SEQ, D_MODEL), 1.0),
        'cache_kv_latent': nrm((L, n_pool, PAGE_SIZE, MLA_KV_LORA), 1.0),
        'cache_k_rope': nrm((L, n_pool, PAGE_SIZE, MLA_ROPE), 1.0),
        'state_C': nrm((L, DEC_BATCH, ML_HEADS, ML_DH, ML_DH), 0.5),
        'state_n': nrm((L, DEC_BATCH, ML_HEADS, ML_DH), 0.5),
        'state_m': nrm((L, DEC_BATCH, ML_HEADS), 1.0),
        'cache_mem_k': nrm((L, DEC_BATCH, N_MEM, MEM_HEADS, MEM_HD), 1.0),
        'cache_mem_v': nrm((L, DEC_BATCH, N_MEM, MEM_HEADS, MEM_HD), 1.0),
        'page_table': page_table,
        'mem_prompt': nrm((BATCH, N_MEM, D_MODEL), 1.0),
        'ln0_g': gain((D_MODEL,)),
        'ln0_b': nrm((D_MODEL,), 0.01),
        'w_in': nrm((L, D_MODEL, IN_TOTAL), D_MODEL ** -0.5) * col_scale,
        'b_i': nrm((L, ML_HEADS), 0.1),
        'b_f': 3.0 + 3.0 * jax.random.uniform(next(ks), (L, ML_HEADS), jnp.float32),
        'g_q': gain((L, MLA_Q_LORA)),
        'w_uq': nrm((L, MLA_Q_LORA, MLA_HEADS, MLA_NOPE + MLA_ROPE), MLA_Q_LORA ** -0.5),
        'g_kv': gain((L, MLA_KV_LORA)),
        'w_uk': nrm((L, MLA_KV_LORA, MLA_HEADS, MLA_NOPE), MLA_KV_LORA ** -0.5),
        'w_uv': nrm((L, MLA_KV_LORA, MLA_HEADS, MLA_V), BETA * MLA_KV_LORA ** -0.5),
        'w_out': nrm((L, D_MIX, D_MODEL), BETA * D_MIX ** -0.5),
        'ln1_g': gain((L, D_MODEL)),
        'ln1_b': nrm((L, D_MODEL), 0.01),
        'w_mq': nrm((L, D_MODEL, MEM_HEADS, MEM_HD), D_MODEL ** -0.5),
        'w_mk': nrm((L, D_MODEL, MEM_HEADS, MEM_HD), D_MODEL ** -0.5),
        'w_mv': nrm((L, D_MODEL, MEM_HEADS, MEM_HD), BETA * D_MODEL ** -0.5),
        'w_mo': nrm((L, MEM_HEADS, MEM_HD, D_MODEL), BETA * D_MODEL ** -0.5),
        'ln2_g': gain((L, D_MODEL)),
        'ln2_b': nrm((L, D_MODEL), 0.01),
        'w_pq': nrm((L, D_MODEL, PEER_HEADS, PEER_DKEY), D_MODEL ** -0.5),
        'sub_k1': nrm((L, PEER_NKEYS, PEER_DKEY // 2), (PEER_DKEY // 2) ** -0.5),
        'sub_k2': nrm((L, PEER_NKEYS, PEER_DKEY // 2), (PEER_DKEY // 2) ** -0.5),
        'peer_u': nrm((L, PEER_N, D_MODEL), D_MODEL ** -0.5),
        'peer_v': nrm((L, PEER_N, D_MODEL), BETA * PEER_HEADS ** -0.5),
        'ln3_g': gain((L, D_MODEL)),
        'ln3_b': nrm((L, D_MODEL), 0.01),
    }


def reference(x_prompt, x_sample, cache_kv_latent, cache_k_rope, state_C, state_n, state_m,
              cache_mem_k, cache_mem_v, page_table, mem_prompt, ln0_g, ln0_b, w_in, b_i, b_f,
              g_q, w_uq, g_kv, w_uk, w_uv, w_out, ln1_g, ln1_b, w_mq, w_mk, w_mv, w_mo,
              ln2_g, ln2_b, w_pq, sub_k1, sub_k2, peer_u, peer_v, ln3_g, ln3_b):
    S = x_prompt.shape[1]
    T = x_sample.shape[1]
    past = page_table.shape[1] * PAGE_SIZE
    pos_p = jnp.arange(S)
    pos_s = past + jnp.arange(T)
    xp = layer_norm(x_prompt, ln0_g, ln0_b)
    xs = layer_norm(x_sample, ln0_g, ln0_b)
    kvl_p, kr_p, C_p, n_p, m_p, mk_p, mv_p = [], [], [], [], [], [], []
    kvl_s, kr_s, C_s, n_s, m_s = [], [], [], [], []
    for l in range(DEPTH):
        lw = {'w_in': w_in[l], 'b_i': b_i[l], 'b_f': b_f[l], 'g_q': g_q[l], 'w_uq': w_uq[l],
              'g_kv': g_kv[l], 'w_uk': w_uk[l], 'w_out': w_out[l], 'ln1_g': ln1_g[l], 'ln1_b': ln1_b[l],
              'w_mq': w_mq[l], 'w_mo': w_mo[l], 'ln2_g': ln2_g[l], 'ln2_b': ln2_b[l], 'w_pq': w_pq[l],
              'sub_k1': sub_k1[l], 'sub_k2': sub_k2[l], 'peer_u': peer_u[l], 'peer_v': peer_v[l],
              'ln3_g': ln3_g[l], 'ln3_b': ln3_b[l]}
        (ql, qr, kvl, kr), ml_in, og = mixer_projections(xp, pos_p, lw)
        mla_o = mla_attend_prompt(ql, qr, kvl, kr, w_uv[l])
        ml_h, (C, n, m) = mlstm_prompt(*ml_in)
        mk, mv = mem_kv(mem_prompt, w_mk[l], w_mv[l])
        xp = finish_layer(xp, mla_o, ml_h, og, mk, mv, lw)
        kvl_p.append(kvl); kr_p.append(kr); C_p.append(C); n_p.append(n); m_p.append(m)
        mk_p.append(mk); mv_p.append(mv)
        (ql, qr, kvl, kr), ml_in, og = mixer_projections(xs, pos_s, lw)
        mla_o = mla_attend_sample(ql, qr, kvl, kr, cache_kv_latent[l], cache_k_rope[l], page_table, w_uv[l])
        carry = (state_C[l].astype(jnp.float32), state_n[l].astype(jnp.float32), state_m[l].astype(jnp.float32))
        (C, n, m), ml_h = mlstm_chunk(carry, ml_in)
        xs = finish_layer(xs, mla_o, ml_h, og, cache_mem_k[l], cache_mem_v[l], lw)
        kvl_s.append(kvl); kr_s.append(kr); C_s.append(C); n_s.append(n); m_s.append(m)
    return (xp, xs, jnp.stack(kvl_p), jnp.stack(kr_p), jnp.stack(C_p), jnp.stack(n_p), jnp.stack(m_p),
            jnp.stack(mk_p), jnp.stack(mv_p), jnp.stack(kvl_s), jnp.stack(kr_s), jnp.stack(C_s),
            jnp.stack(n_s), jnp.stack(m_s))
```

```python
import numpy as np
import concourse.bass as bass
import concourse.mybir as mybir
from concourse.bass_utils import run_bass_kernel_spmd

F32 = mybir.dt.float32
BF16 = mybir.dt.bfloat16
I32 = mybir.dt.int32
U32 = mybir.dt.uint32
ALU = mybir.AluOpType
AF = mybir.ActivationFunctionType
AX = mybir.AxisListType

_DT_SIZE = {F32: 4, BF16: 2, I32: 4, U32: 4}


class Buf:
    __slots__ = ("t", "name", "lo", "hi", "space")

    def __init__(self, t, name, lo=0, hi=0, space="sb"):
        self.t = t
        self.name = name
        self.lo = lo
        self.hi = hi
        self.space = space


class Prog:
    NSEM_C = 4
    NSEM_D = 8

    def __init__(self, nc, sb_base=20480, sb_limit=229376):
        self.nc = nc
        self.ops = []
        self.res_w = {}
        self.res_r = {}
        self.sb_off = sb_base
        self.sb_limit = sb_limit
        self.sb_peak = sb_base
        self.live = []
        self.dead = []
        self.uid = 0

    def sb(self, name, shape, dtype):
        per = 1
        for s in shape[1:]:
            per *= s
        nbytes = per * _DT_SIZE[dtype]
        nbytes = (nbytes + 63) // 64 * 64
        lo = self.sb_off
        hi = lo + nbytes
        assert hi <= self.sb_limit, f"SBUF overflow allocating {name}: {hi} > {self.sb_limit}"
        self.sb_off = hi
        self.sb_peak = max(self.sb_peak, hi)
        self.uid += 1
        t = self.nc.alloc_sbuf_tensor_at(f"{name}_{self.uid}", list(shape), dtype, offset=lo)
        b = Buf(t, name, lo, hi)
        inh = set()
        keep = []
        for (dlo, dhi, s) in self.dead:
            if dlo < hi and lo < dhi:
                inh |= s
                if dlo < lo or dhi > hi:
                    keep.append((dlo, dhi, s))
            else:
                keep.append((dlo, dhi, s))
        self.dead = keep
        if inh:
            self.res_r[b] = sorted(inh)
        self.live.append(b)
        return b

    def mark(self):
        return (self.sb_off, len(self.live))

    def release(self, mark):
        off, nlive = mark
        freed = self.live[nlive:]
        fset = set(id(b) for b in freed)
        acc = {id(b): set() for b in freed}
        for k in list(self.res_w.keys()):
            kb = k[0] if isinstance(k, tuple) else k
            if id(kb) in fset:
                acc[id(kb)].add(self.res_w.pop(k))
        for k in list(self.res_r.keys()):
            kb = k[0] if isinstance(k, tuple) else k
            if id(kb) in fset:
                acc[id(kb)] |= set(self.res_r.pop(k))
        for b in freed:
            if acc[id(b)]:
                self.dead.append((b.lo, b.hi, acc[id(b)]))
        del self.live[nlive:]
        self.sb_off = off

    def ps(self, name, shape, dtype):
        self.uid += 1
        t = self.nc.alloc_psum_tensor(f"{name}_{self.uid}", list(shape), dtype)
        return Buf(t, name, space="ps")

    def op(self, eng, fn, reads=(), writes=(), dma=False):
        i = len(self.ops)
        deps = set()
        for r in reads:
            if r in self.res_w:
                deps.add(self.res_w[r])
            if isinstance(r, tuple) and r[0] in self.res_w:
                deps.add(self.res_w[r[0]])
        for w in writes:
            if w in self.res_w:
                deps.add(self.res_w[w])
            for rd in self.res_r.get(w, ()):
                deps.add(rd)
            if isinstance(w, tuple):
                if w[0] in self.res_w:
                    deps.add(self.res_w[w[0]])
                for rd in self.res_r.get(w[0], ()):
                    deps.add(rd)
        for r in reads:
            self.res_r.setdefault(r, []).append(i)
        for w in writes:
            self.res_w[w] = i
            self.res_r[w] = []
        deps.discard(i)
        self.ops.append(dict(eng=eng, fn=fn, deps=deps, dma=dma))
        return i

    def dma(self, q, out, in_, reads=(), writes=(), **kw):
        return self.op(q, lambda e: e.dma_start(out=out, in_=in_, **kw), reads=reads, writes=writes, dma=True)

    def make_identity(self, ident, dtype_is_bf16=True):
        t = ident.t
        self.op('pool', lambda e: e.memset(t[:, :], 1.0), writes=[ident])
        self.op('pool', lambda e: e.affine_select(t[:, :], t[:, :], [[-1, 128]], ALU.is_equal, 0.0,
                                                  base=0, channel_multiplier=1),
                reads=[ident], writes=[ident])

    def emit(self, final_wait=()):
        nc = self.nc
        ops = self.ops
        n = len(ops)
        need = [False] * n
        for i, o in enumerate(ops):
            for d in o['deps']:
                od = ops[d]
                if od['eng'] == 'pe' and o['eng'] == 'pe' and not od['dma'] and not o['dma']:
                    continue
                need[d] = True
        final_ops = []
        for r in final_wait:
            if r in self.res_w:
                final_ops.append(self.res_w[r])
                need[self.res_w[r]] = True
        engs = ['pe', 'act', 'dve', 'pool', 'sp']
        ccount = {e: 0 for e in engs}
        dcount = {e: 0 for e in engs}
        sig = [None] * n
        for i, o in enumerate(ops):
            e = o['eng']
            if o['dma']:
                d = dcount[e]
                dcount[e] += 1
                sig[i] = ('d', e, d)
            elif need[i]:
                c = ccount[e]
                ccount[e] += 1
                sig[i] = ('c', e, c)
        import contextlib
        with contextlib.ExitStack() as es:
            csem = {e: [es.enter_context(nc.semaphore(f"c_{e}_{k}")) for k in range(self.NSEM_C)]
                    for e in engs if ccount[e] > 0}
            dsem = {e: [es.enter_context(nc.semaphore(f"d_{e}_{k}")) for k in range(self.NSEM_D)]
                    for e in engs if dcount[e] > 0}
            per_eng = {e: [i for i, o in enumerate(ops) if o['eng'] == e] for e in engs}
            if final_ops:
                pass
            NC_, ND_ = self.NSEM_C, self.NSEM_D

            def run_engine(ename, eobj):
                waited_c = {}
                waited_d = {}
                def wait_sig(s):
                    kind, pe_, c = s
                    if kind == 'c':
                        if waited_c.get(pe_, -1) >= c:
                            return
                        waited_c[pe_] = c
                        eobj.wait_ge(csem[pe_][c % NC_], c // NC_ + 1)
                    else:
                        key = (pe_, c % ND_)
                        val = 16 * (c // ND_ + 1)
                        if waited_d.get(key, 0) >= val:
                            return
                        waited_d[key] = val
                        eobj.wait_ge(dsem[pe_][c % ND_], val)
                for i in per_eng[ename]:
                    o = ops[i]
                    for d in sorted(o['deps']):
                        od = ops[d]
                        if od['eng'] == 'pe' and ename == 'pe' and not od['dma'] and not o['dma']:
                            continue
                        wait_sig(sig[d])
                    if o['dma']:
                        _, q, dnum = sig[i]
                        if dnum >= ND_:
                            wait_sig(('d', q, dnum - ND_))
                    ins = o['fn'](eobj)
                    s = sig[i]
                    if s is not None:
                        if s[0] == 'c':
                            ins.then_inc(csem[s[1]][s[2] % NC_], 1)
                        else:
                            ins.then_inc(dsem[s[1]][s[2] % ND_], 16)
                if ename == 'sp':
                    for f in final_ops:
                        wait_sig(sig[f])

            with nc.Block() as block:
                @block.tensor
                def _(e):
                    run_engine('pe', e)

                @block.scalar
                def _(e):
                    run_engine('act', e)

                @block.vector
                def _(e):
                    run_engine('dve', e)

                @block.gpsimd
                def _(e):
                    run_engine('pool', e)

                @block.sync
                def _(e):
                    run_engine('sp', e)


NT = 17
NROW = NT * 128
D = 1024
NPOOL_ROWS = 10240 * 128
MLA_SCALE = 96.0 ** -0.5
NEG = -1.0e30
_DBG = None
_PH3 = False
PI = float(np.pi)
ALPHA = 2.0 ** 0.25


def build_nc(stop_after=99):
    nc = bass.Bass("TRN2", target_bir_lowering=False)

    def din(name, shape, dt=F32):
        return nc.dram_tensor(name, list(shape), dt, kind="ExternalInput").ap()

    def dout(name, shape, dt=F32):
        return nc.dram_tensor(name, list(shape), dt, kind="ExternalOutput").ap()

    def dint(name, shape, dt=F32):
        return nc.dram_tensor(name, list(shape), dt, kind="Internal").ap()

    xin = din("xin", [NROW, D])
    ckv = din("ckv", [NPOOL_ROWS, 256]) if _PH3 else None
    ckr = din("ckr", [NPOOL_ROWS, 32]) if _PH3 else None
    stC = din("stC", [16, 4, 128, 128])
    stn = din("stn", [16, 4, 128])
    stm = din("stm", [16, 4])
    cmk = din("cmk", [16, 256, 1024])
    cmv = din("cmv", [16, 256, 1024])
    ptab = din("ptab", [1024], I32)
    memp = din("memp", [256, D])
    ropeinv = din("ropeinv", [16])
    ln_g = [din(f"ln{i}_g", [D]) for i in range(4)]
    ln_b = [din(f"ln{i}_b", [D]) for i in range(4)]
    w_in = din("w_in", [D, 2728])
    b_i = din("b_i", [4])
    b_f = din("b_f", [4])
    g_q = din("g_q", [384])
    w_uq = din("w_uq", [384, 768])
    g_kv = din("g_kv", [256])
    w_uk = din("w_uk", [256, 512])
    w_ukT = din("w_ukT", [64, 8 * 256])
    w_uv = din("w_uv", [256, 512])
    w_out = din("w_out", [D, D])
    w_mq = din("w_mq", [D, D])
    w_mk = din("w_mk", [D, D])
    w_mv = din("w_mv", [D, D])
    w_mo = din("w_mo", [D, D])
    w_pq = din("w_pq", [D, D])
    k1T = din("k1T", [64, 128])
    k2T = din("k2T", [64, 128])
    peer_u = din("peer_u", [16384, D])
    peer_v = din("peer_v", [16384, D])

    y_o = dout("y", [NROW, D])
    kvl_o = dout("kvl", [NROW, 256])
    kr_o = dout("kr", [NROW, 32])
    Cp_o = dout("Cp", [4, 128, 128])
    np_o = dout("np_", [4, 128])
    mp_o = dout("mp", [1, 4])
    mkp_o = dout("mkp", [256, D])
    mvp_o = dout("mvp", [256, D])
    Cs_o = dout("Cs", [16, 4, 128, 128])
    ns_o = dout("ns", [16, 4, 128])
    ms_o = dout("ms", [16, 4])

    XN = dint("XN", [NROW, D])
    ML = dint("ML", [NROW, 2056])
    MIX = dint("MIX", [NROW, D])
    X1 = dint("X1", [NROW, D])
    X2 = dint("X2", [NROW, D])
    dbg_o = dout("dbg", [NROW, D]) if _DBG else None

    P = Prog(nc)
    V = lambda fn, r=(), w=(): P.op('dve', fn, r, w)
    A = lambda fn, r=(), w=(): P.op('act', fn, r, w)
    G = lambda fn, r=(), w=(): P.op('pool', fn, r, w)
    T = lambda fn, r=(), w=(): P.op('pe', fn, r, w)

    psf = [P.ps(f"psf{i}", [128, 512], F32) for i in range(4)]
    psS = P.ps("psS", [128, 1024], F32)
    psb = [P.ps(f"psb{i}", [128, 1024], BF16) for i in range(2)]
    cnt = {'f': 0, 'b': 0}

    def nf():
        cnt['f'] += 1
        return psf[cnt['f'] % 4]

    def nb():
        cnt['b'] += 1
        return psb[cnt['b'] % 2]

    ident = P.sb("ident", [128, 128], BF16)
    P.make_identity(ident)
    maskc = P.sb("maskc", [128, 128], F32)
    G(lambda e: e.memset(maskc.t[:, :], 0.0), (), [maskc])
    G(lambda e: e.affine_select(maskc.t[:, :], maskc.t[:, :], [[-1, 128]], ALU.is_ge, NEG, base=0, channel_multiplier=1),
      [maskc], [maskc])
    posi = P.sb("posi", [128, NT], I32)
    posf = P.sb("posf", [128, NT], F32)
    inv_t = P.sb("inv_t", [128, 16], F32)
    ang = P.sb("ang", [128, NT, 16], F32)
    cos_t = P.sb("cos_t", [128, NT, 16], F32)
    sin_t = P.sb("sin_t", [128, NT, 16], F32)
    G(lambda e: e.iota(posi.t[:, :], [[128, NT]], base=0, channel_multiplier=1), (), [posi])
    V(lambda e: e.tensor_single_scalar(posi.t[:, 16:17], posi.t[:, 0:1], 3, ALU.bitwise_and), [posi], [posi])
    V(lambda e: e.tensor_copy(posf.t[:, :], posi.t[:, :]), [posi], [posf])
    V(lambda e: e.tensor_scalar(posf.t[:, 16:17], posf.t[:, 16:17], 8192.0, None, ALU.add), [posf], [posf])
    P.dma('sp', inv_t.t[:, :], ropeinv.partition_broadcast(128), (), [inv_t])
    V(lambda e: e.tensor_tensor(ang.t[:, :, :], posf.t[:, :].unsqueeze(2).broadcast_to([128, NT, 16]),
                                inv_t.t[:, :].unsqueeze(1).broadcast_to([128, NT, 16]), ALU.mult), [posf, inv_t], [ang])
    ki = P.sb("ki", [128, NT, 16], I32)
    kf = P.sb("kf", [128, NT, 16], F32)
    rr = P.sb("rr", [128, NT, 16], F32)
    C1 = 6.28125
    C2 = 2 * PI - C1

    def sin_of(dst, shift):
        V(lambda e: e.tensor_scalar(rr.t[:, :, :], ang.t[:, :, :], shift, 1.0 / (2 * PI), ALU.add, ALU.mult), [ang], [rr])
        V(lambda e: e.tensor_copy(ki.t[:, :, :], rr.t[:, :, :]), [rr], [ki])
        V(lambda e: e.tensor_copy(kf.t[:, :, :], ki.t[:, :, :]), [ki], [kf])
        V(lambda e: e.tensor_scalar(rr.t[:, :, :], ang.t[:, :, :], shift, None, ALU.add), [ang], [rr])
        V(lambda e: e.scalar_tensor_tensor(rr.t[:, :, :], kf.t[:, :, :], -C1, rr.t[:, :, :], ALU.mult, ALU.add), [kf, rr], [rr])
        V(lambda e: e.scalar_tensor_tensor(rr.t[:, :, :], kf.t[:, :, :], -C2, rr.t[:, :, :], ALU.mult, ALU.add), [kf, rr], [rr])
        V(lambda e: e.tensor_scalar(kf.t[:, :, :], rr.t[:, :, :], PI, -2 * PI, ALU.is_gt, ALU.mult), [rr], [kf])
        V(lambda e: e.tensor_tensor(rr.t[:, :, :], rr.t[:, :, :], kf.t[:, :, :], ALU.add), [rr, kf], [rr])
        V(lambda e: e.tensor_scalar(kf.t[:, :, :], rr.t[:, :, :], -PI, 2 * PI, ALU.is_lt, ALU.mult), [rr], [kf])
        V(lambda e: e.tensor_tensor(rr.t[:, :, :], rr.t[:, :, :], kf.t[:, :, :], ALU.add), [rr, kf], [rr])
        V(lambda e: e.tensor_scalar(rr.t[:, :, :], rr.t[:, :, :], PI, -PI, ALU.min, ALU.max), [rr], [rr])
        A(lambda e: e.activation(dst.t[:, :, :], rr.t[:, :, :], AF.Sin), [rr], [dst])

    sin_of(sin_t, 0.0)
    sin_of(cos_t, PI / 2)

    st6 = P.sb("st6", [128, 2, 6], F32)
    mv2 = P.sb("mv2", [128, 2], F32)
    rs1 = P.sb("rs1", [128, 1], F32)
    nm1 = P.sb("nm1", [128, 1], F32)
    junk = P.sb("junk", [128, 1024], F32)

    def layernorm(x, g_t, b_t, eps=1e-5):
        for c in range(2):
            V(lambda e, c=c: e.bn_stats(st6.t[:, c, :], x.t[:, c * 512:(c + 1) * 512]), [x], [st6])
        V(lambda e: e.bn_aggr(mv2.t[:, :], st6.t[:, :, :]), [st6], [mv2])
        V(lambda e: e.tensor_scalar(rs1.t[:, :], mv2.t[:, 1:2], eps, None, ALU.add), [mv2], [rs1])
        A(lambda e: e.sqrt(rs1.t[:, :], rs1.t[:, :]), [rs1], [rs1])
        V(lambda e: e.reciprocal(rs1.t[:, :], rs1.t[:, :]), [rs1], [rs1])
        V(lambda e: e.tensor_scalar(nm1.t[:, :], mv2.t[:, 0:1], rs1.t[:, 0:1], -1.0, ALU.mult, ALU.mult), [mv2, rs1], [nm1])
        A(lambda e: e.activation(x.t[:, :], x.t[:, :], AF.Identity, bias=nm1.t[:, 0:1], scale=rs1.t[:, 0:1]), [x, nm1, rs1], [x])
        V(lambda e: e.tensor_tensor(x.t[:, :], x.t[:, :], g_t.t[:, :], ALU.mult), [x, g_t], [x])
        V(lambda e: e.tensor_tensor(x.t[:, :], x.t[:, :], b_t.t[:, :], ALU.add), [x, b_t], [x])

    ss1 = P.sb("ss1", [128, 1], F32)

    def rmsnorm(out_ap, out_buf, x_ap, x_buf, W, g_ap, g_buf, eps=1e-6):
        A(lambda e: e.activation(junk.t[:, 0:W], x_ap, AF.Square, accum_out=ss1.t[:, 0:1]), [x_buf], [junk, ss1])
        V(lambda e: e.tensor_scalar(ss1.t[:, :], ss1.t[:, :], 1.0 / W, eps, ALU.mult, ALU.add), [ss1], [ss1])
        A(lambda e: e.sqrt(ss1.t[:, :], ss1.t[:, :]), [ss1], [ss1])
        V(lambda e: e.reciprocal(ss1.t[:, :], ss1.t[:, :]), [ss1], [ss1])
        V(lambda e: e.scalar_tensor_tensor(out_ap, x_ap, ss1.t[:, 0:1], g_ap, ALU.mult, ALU.mult), [x_buf, ss1, g_buf], [out_buf])

    def transposes(dst_ap_fn, dst_buf, src_fn, src_buf, n, rows=128, cols=128):
        pb = nb()
        for i in range(n):
            T(lambda e, i=i: e.transpose(pb.t[0:cols, i * 128:i * 128 + rows], src_fn(i), ident.t[0:rows, 0:rows]),
              [src_buf, ident], [pb])
        return pb

    rope_tmp = P.sb("rope_tmp", [128, 4, 8, 16], F32)

    def rope(x_buf, x1_ap, x2_ap, nh, tt):
        c = cos_t.t[:, tt, :].unsqueeze(1).broadcast_to([128, nh, 16])
        s = sin_t.t[:, tt, :].unsqueeze(1).broadcast_to([128, nh, 16])
        t = [rope_tmp.t[:, i, 0:nh, :] for i in range(4)]
        V(lambda e: e.tensor_tensor(t[0], x1_ap, c, ALU.mult), [x_buf, cos_t], [rope_tmp])
        V(lambda e: e.tensor_tensor(t[1], x2_ap, s, ALU.mult), [x_buf, sin_t], [rope_tmp])
        V(lambda e: e.tensor_tensor(t[2], x1_ap, s, ALU.mult), [x_buf, sin_t], [rope_tmp])
        V(lambda e: e.tensor_tensor(t[3], x2_ap, c, ALU.mult), [x_buf, cos_t], [rope_tmp])
        V(lambda e: e.tensor_tensor(x1_ap, t[0], t[1], ALU.subtract), [rope_tmp], [x_buf])
        V(lambda e: e.tensor_tensor(x2_ap, t[2], t[3], ALU.add), [rope_tmp], [x_buf])

    QTs = P.sb("QTs", [128, 8, 128], BF16)
    kvT_s = P.sb("kvT_s", [128, 2, 128], BF16)
    KnT = P.sb("KnT", [128, 128], BF16)
    markA = P.mark()
    QT = P.sb("QT", [128, 8, 2048], BF16)
    KT = P.sb("KT", [128, 8, 2048], BF16)
    Vh = P.sb("Vh", [128, 16, 512], BF16)
    markB = P.mark()

    w_in_sb = P.sb("w_in_sb", [128, 8, 2728], BF16)
    P.dma('pool', w_in_sb.t[:, :, :], w_in.rearrange("(c p) n -> p c n", p=128), (), [w_in_sb])
    w_uq_sb = P.sb("w_uq_sb", [128, 3, 768], BF16)
    P.dma('pool', w_uq_sb.t[:, :, :], w_uq.rearrange("(c p) n -> p c n", p=128), (), [w_uq_sb])
    w_uk_sb = P.sb("w_uk_sb", [128, 2, 512], BF16)
    P.dma('pool', w_uk_sb.t[:, :, :], w_uk.rearrange("(c p) n -> p c n", p=128), (), [w_uk_sb])
    w_uv_sb = P.sb("w_uv_sb", [128, 2, 512], BF16)
    P.dma('pool', w_uv_sb.t[:, :, :], w_uv.rearrange("(c p) n -> p c n", p=128), (), [w_uv_sb])
    g0 = P.sb("g0", [128, D], F32)
    b0 = P.sb("b0", [128, D], F32)
    P.dma('sp', g0.t[:, :], ln_g[0].partition_broadcast(128), (), [g0])
    P.dma('sp', b0.t[:, :], ln_b[0].partition_broadcast(128), (), [b0])
    gq_t = P.sb("gq_t", [128, 384], F32)
    gkv_t = P.sb("gkv_t", [128, 256], F32)
    bi_t = P.sb("bi_t", [128, 4], F32)
    bf_t = P.sb("bf_t", [128, 4], F32)
    P.dma('sp', gq_t.t[:, :], g_q.partition_broadcast(128), (), [gq_t])
    P.dma('sp', gkv_t.t[:, :], g_kv.partition_broadcast(128), (), [gkv_t])
    P.dma('sp', bi_t.t[:, :], b_i.partition_broadcast(128), (), [bi_t])
    P.dma('sp', bf_t.t[:, :], b_f.partition_broadcast(128), (), [bf_t])

    xts = [P.sb(f"xt{i}", [128, D], F32) for i in range(2)]
    xb = P.sb("xb", [128, D], BF16)
    xT = P.sb("xT", [128, 8, 128], BF16)
    z = P.sb("z", [128, 2728], F32)
    cqn = P.sb("cqn", [128, 384], BF16)
    cqT = P.sb("cqT", [128, 3, 128], BF16)
    q_sb = P.sb("q_sb", [128, 8, 96], F32)
    qb = P.sb("qb", [128, 8, 96], BF16)
    kvl = P.sb("kvl", [128, 256], F32)
    kvb = P.sb("kvb", [128, 256], BF16)
    kvT = P.sb("kvT", [128, 2, 128], BF16)
    kr = P.sb("kr", [128, 32], F32)
    kfull = P.sb("kfull", [128, 8, 96], BF16)
    mlt = P.sb("mlt", [128, 2056], F32)
    ft = P.sb("ft", [128, 4, 4], F32)

    for tt in range(NT):
        r0 = tt * 128
        xt = xts[tt % 2]
        P.dma('sp', xt.t[:, :], xin[r0:r0 + 128, :], (), [xt])
        layernorm(xt, g0, b0)
        P.dma('sp', XN[r0:r0 + 128, :], xt.t[:, :], [xt], [('XN', tt)])
        A(lambda e, xt=xt: e.copy(xb.t[:, :], xt.t[:, :]), [xt], [xb])
        pb = transposes(None, None, lambda i: xb.t[:, i * 128:(i + 1) * 128], xb, 8)
        V(lambda e, pb=pb: e.tensor_copy(xT.t[:, :, :], pb.t[:, :].rearrange("p (c t) -> p c t", c=8)), [pb], [xT])
        for nci, n0 in enumerate(range(0, 2728, 512)):
            nw = min(512, 2728 - n0)
            pz = nf()
            for c in range(8):
                T(lambda e, c=c, pz=pz, n0=n0, nw=nw: e.matmul(pz.t[:, 0:nw], xT.t[:, c, :], w_in_sb.t[:, c, n0:n0 + nw],
                                                              start=(c == 0), stop=(c == 7)), [xT, w_in_sb], [pz])
            if nci % 2 == 0:
                A(lambda e, pz=pz, n0=n0, nw=nw: e.copy(z.t[:, n0:n0 + nw], pz.t[:, 0:nw]), [pz], [z])
            else:
                V(lambda e, pz=pz, n0=n0, nw=nw: e.tensor_copy(z.t[:, n0:n0 + nw], pz.t[:, 0:nw]), [pz], [z])
        rmsnorm(cqn.t[:, :], cqn, z.t[:, 0:384], z, 384, gq_t.t[:, :], gq_t)
        pb = transposes(None, None, lambda i: cqn.t[:, i * 128:(i + 1) * 128], cqn, 3)
        V(lambda e, pb=pb: e.tensor_copy(cqT.t[:, :, :], pb.t[:, 0:384].rearrange("p (c t) -> p c t", c=3)), [pb], [cqT])
        for (n0, nw) in ((0, 512), (512, 256)):
            pz = nf()
            for c in range(3):
                T(lambda e, c=c, pz=pz, n0=n0, nw=nw: e.matmul(pz.t[:, 0:nw], cqT.t[:, c, :], w_uq_sb.t[:, c, n0:n0 + nw],
                                                              start=(c == 0), stop=(c == 2)), [cqT, w_uq_sb], [pz])
            V(lambda e, pz=pz, n0=n0, nw=nw: e.tensor_copy(q_sb.t[:, :, :].rearrange("p h d -> p (h d)")[:, n0:n0 + nw], pz.t[:, 0:nw]),
              [pz], [q_sb])
        rope(q_sb, q_sb.t[:, :, 64:80], q_sb.t[:, :, 80:96], 8, tt)
        A(lambda e: e.copy(qb.t[:, :, :], q_sb.t[:, :, :]), [q_sb], [qb])
        pb = transposes(None, None, lambda i: qb.t[:, i, :], qb, 8, rows=128, cols=96)
        if tt < 16:
            V(lambda e, pb=pb, r0=r0: e.tensor_copy(QT.t[0:96, :, r0:r0 + 128], pb.t[0:96, :].rearrange("p (c t) -> p c t", c=8)),
              [pb], [(QT, tt)])
        else:
            V(lambda e, pb=pb: e.tensor_copy(QTs.t[0:96, :, :], pb.t[0:96, :].rearrange("p (c t) -> p c t", c=8)), [pb], [QTs])
        rmsnorm(kvl.t[:, :], kvl, z.t[:, 384:640], z, 256, gkv_t.t[:, :], gkv_t)
        P.dma('sp', kvl_o[r0:r0 + 128, :], kvl.t[:, :], [kvl], ['kvl_o'])
        A(lambda e: e.copy(kvb.t[:, :], kvl.t[:, :]), [kvl], [kvb])
        A(lambda e: e.copy(kr.t[:, :], z.t[:, 640:672]), [z], [kr])
        rope(kr, kr.t[:, 0:16].unsqueeze(1), kr.t[:, 16:32].unsqueeze(1), 1, tt)
        P.dma('sp', kr_o[r0:r0 + 128, :], kr.t[:, :], [kr], ['kr_o'])
        pb = transposes(None, None, lambda i: kvb.t[:, i * 128:(i + 1) * 128], kvb, 2)
        if tt < 16:
            V(lambda e, pb=pb: e.tensor_copy(kvT.t[:, :, :], pb.t[:, 0:256].rearrange("p (c t) -> p c t", c=2)), [pb], [kvT])
            pk = nf()
            for c in range(2):
                T(lambda e, c=c, pk=pk: e.matmul(pk.t[:, :], kvT.t[:, c, :], w_uk_sb.t[:, c, :], start=(c == 0), stop=(c == 1)),
                  [kvT, w_uk_sb], [pk])
            V(lambda e, pk=pk: e.tensor_copy(kfull.t[:, :, 0:64], pk.t[:, :].rearrange("p (h d) -> p h d", h=8)), [pk], [kfull])
            A(lambda e: e.copy(kfull.t[:, :, 64:96], kr.t[:, :].unsqueeze(1).broadcast_to([128, 8, 32])), [kr], [kfull])
            pb = transposes(None, None, lambda i: kfull.t[:, i, :], kfull, 8, rows=128, cols=96)
            V(lambda e, pb=pb, r0=r0: e.tensor_copy(KT.t[0:96, :, r0:r0 + 128], pb.t[0:96, :].rearrange("p (c t) -> p c t", c=8)),
              [pb], [(KT, tt)])
            pv = nf()
            for c in range(2):
                T(lambda e, c=c, pv=pv: e.matmul(pv.t[:, :], kvT.t[:, c, :], w_uv_sb.t[:, c, :], start=(c == 0), stop=(c == 1)),
                  [kvT, w_uv_sb], [pv])
            A(lambda e, pv=pv, tt=tt: e.copy(Vh.t[:, tt, :], pv.t[:, :]), [pv], [(Vh, tt)])
        else:
            V(lambda e, pb=pb: e.tensor_copy(kvT_s.t[:, :, :], pb.t[:, 0:256].rearrange("p (c t) -> p c t", c=2)), [pb], [kvT_s])
            A(lambda e: e.copy(kfull.t[:, 0, 64:96], kr.t[:, :]), [kr], [kfull])
            pb = transposes(None, None, lambda i: kfull.t[:, 0, :], kfull, 1, rows=128, cols=96)
            V(lambda e, pb=pb: e.tensor_copy(KnT.t[64:96, :], pb.t[64:96, 0:128]), [pb], [KnT])
        A(lambda e: e.copy(mlt.t[:, 0:512], z.t[:, 672:1184]), [z], [mlt])
        V(lambda e: e.tensor_scalar(mlt.t[:, 512:1024], z.t[:, 1184:1696], 128.0 ** -0.5, None, ALU.mult), [z], [mlt])
        A(lambda e: e.copy(mlt.t[:, 1024:1536], z.t[:, 1696:2208]), [z], [mlt])
        A(lambda e: e.activation(mlt.t[:, 1536:2048], z.t[:, 2216:2728], AF.Sigmoid), [z], [mlt])
        V(lambda e: e.tensor_tensor(mlt.t[:, 2048:2052], z.t[:, 2208:2212], bi_t.t[:, :], ALU.add), [z, bi_t], [mlt])
        V(lambda e: e.tensor_tensor(ft.t[:, 0, :], z.t[:, 2212:2216], bf_t.t[:, :], ALU.add), [z, bf_t], [ft])
        A(lambda e: e.activation(ft.t[:, 1, :], ft.t[:, 0, :], AF.Abs), [ft], [ft])
        A(lambda e: e.activation(ft.t[:, 1, :], ft.t[:, 1, :], AF.Exp, scale=-1.0), [ft], [ft])
        V(lambda e: e.tensor_scalar(ft.t[:, 1, :], ft.t[:, 1, :], 1.0, None, ALU.add), [ft], [ft])
        A(lambda e: e.activation(ft.t[:, 1, :], ft.t[:, 1, :], AF.Ln), [ft], [ft])
        V(lambda e: e.tensor_scalar(ft.t[:, 2, :], ft.t[:, 0, :], 0.0, None, ALU.min), [ft], [ft])
        V(lambda e: e.tensor_tensor(mlt.t[:, 2052:2056], ft.t[:, 2, :], ft.t[:, 1, :], ALU.subtract), [ft], [mlt])
        P.dma('sp', ML[r0:r0 + 128, :], mlt.t[:, :], [mlt], [('ML', tt)])
    P.release(markB)
    if stop_after <= 1:
        P.emit(final_wait=['kvl_o', 'kr_o'])
        return nc
    p2 = P.mark()
    Pb = [P.sb(f"Pb{i}", [128, 2048], BF16) for i in range(2)]
    PTs = [P.sb(f"PTs{i}", [128, 16, 128], BF16) for i in range(2)]
    mla_sb = [P.sb(f"mla_sb{i}", [128, 512], F32) for i in range(2)]
    mxp = P.sb("mxp", [128, 4], F32)
    mx1 = P.sb("mx1", [128, 1], F32)
    rsp = P.sb("rsp", [128, 4], F32)
    rinv = P.sb("rinv", [128, 1], F32)
    for i in range(16):
        nkt = i + 1
        nch = (nkt + 3) // 4
        msb = mla_sb[i % 2]
        for h in range(8):
            par = h % 2
            pb_, pt_ = Pb[par], PTs[par]
            ws = []
            for j in range(nch):
                w = min(512, nkt * 128 - j * 512)
                ws.append(w)
                T(lambda e, j=j, w=w, h=h, i=i: e.matmul(psf[j].t[:, 0:w], QT.t[0:96, h, i * 128:(i + 1) * 128],
                                                         KT.t[0:96, h, j * 512:j * 512 + w], start=True, stop=True),
                  [(QT, i), (KT, i)], [psf[j]])
            jd, off = i // 4, (i % 4) * 128
            V(lambda e, jd=jd, off=off: e.tensor_tensor(psf[jd].t[:, off:off + 128], psf[jd].t[:, off:off + 128], maskc.t[:, :], ALU.add),
              [psf[jd], maskc], [psf[jd]])
            for j in range(nch):
                V(lambda e, j=j, w=ws[j]: e.tensor_reduce(mxp.t[:, j:j + 1], psf[j].t[:, 0:w], AX.X, ALU.max), [psf[j]], [mxp])
            V(lambda e, nch=nch: e.tensor_reduce(mx1.t[:, :], mxp.t[:, 0:nch], AX.X, ALU.max), [mxp], [mx1])
            V(lambda e: e.tensor_scalar(mx1.t[:, :], mx1.t[:, :], -MLA_SCALE, None, ALU.mult), [mx1], [mx1])
            for j in range(nch):
                A(lambda e, j=j, w=ws[j], pb_=pb_: e.activation(pb_.t[:, j * 512:j * 512 + w], psf[j].t[:, 0:w], AF.Exp,
                                                              bias=mx1.t[:, 0:1], scale=MLA_SCALE, accum_out=rsp.t[:, j:j + 1]),
                  [psf[j], mx1], [pb_, rsp])
            V(lambda e, nch=nch: e.tensor_reduce(rinv.t[:, :], rsp.t[:, 0:nch], AX.X, ALU.add), [rsp], [rinv])
            V(lambda e: e.reciprocal(rinv.t[:, :], rinv.t[:, :]), [rinv], [rinv])
            for g0_ in range(0, nkt, 8):
                gn = min(8, nkt - g0_)
                pbk = nb()
                for k in range(gn):
                    kt = g0_ + k
                    T(lambda e, k=k, kt=kt, pbk=pbk, pb_=pb_: e.transpose(pbk.t[:, k * 128:(k + 1) * 128], pb_.t[:, kt * 128:(kt + 1) * 128], ident.t[:, :]),
                      [pb_, ident], [pbk])
                if (g0_ // 8) % 2 == 0:
                    V(lambda e, pbk=pbk, pt_=pt_, g0_=g0_, gn=gn: e.tensor_copy(pt_.t[:, g0_:g0_ + gn, :], pbk.t[:, 0:gn * 128].rearrange("p (c t) -> p c t", c=gn)),
                      [pbk], [pt_])
                else:
                    A(lambda e, pbk=pbk, pt_=pt_, g0_=g0_, gn=gn: e.copy(pt_.t[:, g0_:g0_ + gn, :], pbk.t[:, 0:gn * 128].rearrange("p (c t) -> p c t", c=gn)),
                      [pbk], [pt_])
            for kt in range(nkt):
                T(lambda e, kt=kt, pt_=pt_, h=h, nkt=nkt: e.matmul(psS.t[:, 0:64], pt_.t[:, kt, :], Vh.t[:, kt, h * 64:(h + 1) * 64],
                                                                 start=(kt == 0), stop=(kt == nkt - 1)), [pt_, (Vh, i)], [psS])
            V(lambda e, h=h, msb=msb: e.tensor_scalar(msb.t[:, h * 64:(h + 1) * 64], psS.t[:, 0:64], rinv.t[:, 0:1], None, ALU.mult),
              [psS, rinv], [msb])
        P.dma('sp', MIX[i * 128:(i + 1) * 128, 0:512], msb.t[:, :], [msb], [('MIXa', i)])
    P.release(p2)
    P.release(markA)

    if _PH3:
        p3 = P.mark()
        w_ukT_sb = P.sb("w_ukT_sb", [64, 8, 256], BF16)
        P.dma('pool', w_ukT_sb.t[:, :, :], w_ukT.rearrange("n (h c) -> n h c", h=8), (), [w_ukT_sb])
        w_uv2 = P.sb("w_uv2", [128, 2, 512], BF16)
        P.dma('pool', w_uv2.t[:, :, :], w_uv.rearrange("(c p) n -> p c n", p=128), (), [w_uv2])
        ptab_bc = P.sb("ptab_bc", [128, 1024], I32)
        P.dma('sp', ptab_bc.t[:, :], ptab.partition_broadcast(128), (), [ptab_bc])
        pidx = P.sb("pidx", [128, 1024], I32)
        G(lambda e: e.iota(pidx.t[:, :], [[0, 1024]], base=0, channel_multiplier=1), (), [pidx])
        off = P.sb("off", [128, 1024], I32)
        V(lambda e: e.tensor_scalar(off.t[:, :], ptab_bc.t[:, :], 128, None, ALU.mult), [ptab_bc], [off])
        V(lambda e: e.tensor_tensor(off.t[:, :], off.t[:, :], pidx.t[:, :], ALU.add), [off, pidx], [off])
        QlatT = P.sb("QlatT", [128, 2, 8, 64], BF16)
        for cc in range(2):
            for h in range(8):
                T(lambda e, cc=cc, h=h: e.matmul(psS.t[:, (cc * 8 + h) * 64:(cc * 8 + h + 1) * 64], w_ukT_sb.t[0:64, h, cc * 128:(cc + 1) * 128],
                                                 QTs.t[0:64, h, 0:64], start=True, stop=True), [w_ukT_sb, QTs], [psS])
        V(lambda e: e.tensor_copy(QlatT.t[:, :, :, :], psS.t[:, :].rearrange("p (a h t) -> p a h t", a=2, h=8)), [psS], [QlatT])
        m4i = P.sb("m4i", [32, 8], I32)
        m4f = P.sb("m4f", [32, 8], F32)
        G(lambda e: e.iota(m4i.t[:, 0:1], [[0, 1]], base=0, channel_multiplier=1), (), [m4i])
        G(lambda e: e.iota(m4i.t[:, 4:8], [[1, 4]], base=0, channel_multiplier=0), (), [m4i])
        V(lambda e: e.tensor_single_scalar(m4i.t[:, 0:1], m4i.t[:, 0:1], 3, ALU.bitwise_and), [m4i], [m4i])
        V(lambda e: e.tensor_copy(m4f.t[:, :], m4i.t[:, :]), [m4i], [m4f])
        V(lambda e: e.tensor_scalar(m4f.t[:, 4:8], m4f.t[:, 4:8], m4f.t[:, 0:1], NEG, ALU.is_gt, ALU.mult), [m4f], [m4f])
        Kb = P.sb("Kb", [128, 64, 256], BF16)
        Kx = P.sb("Kx", [128, 4, 96], BF16)
        G(lambda e: e.memset(Kx.t[:, :, :], 0.0), (), [Kx])
        latf = [P.sb(f"latf{i}", [128, 256], F32) for i in range(4)]
        ropef = [P.sb(f"ropef{i}", [128, 32], F32) for i in range(4)]
        KTc = P.sb("KTc", [128, 2, 512], BF16)
        KrT = P.sb("KrT", [128, 512], BF16)
        S_sb = P.sb("S_sb", [32, 8200], F32)
        Pb16 = P.sb("Pb16", [32, 8200], BF16)
        PTk = P.sb("PTk", [128, 65, 32], BF16)
        kvn_f = P.sb("kvn_f", [4, 256], F32)
        kvn_b = P.sb("kvn_b", [4, 256], BF16)
        smx3 = P.sb("smx3", [32, 1], F32)
        ssum3 = P.sb("ssum3", [32, 1], F32)
        O_sb = P.sb("O_sb", [32, 256], BF16)
        OTb = P.sb("OTb", [128, 2, 32], BF16)
        pm_view = psS.t[:, 512:1024].rearrange("p (h t) -> p h t", h=8)
        for bl in range(16):
            qsl = slice(bl * 4, (bl + 1) * 4)
            for g in range(16):
                for k in range(4):
                    j = g * 4 + k
                    lf_, rf_ = latf[k], ropef[k]
                    G(lambda e, j=j, lf_=lf_, bl=bl: e.indirect_dma_start(out=lf_.t[:, :], out_offset=None, in_=ckv[:, :],
                                                                        in_offset=bass.IndirectOffsetOnAxis(ap=off.t[:, bl * 64 + j:bl * 64 + j + 1], axis=0)),
                      [off], [lf_])
                    P.ops[-1]['dma'] = True
                    G(lambda e, j=j, rf_=rf_, bl=bl: e.indirect_dma_start(out=rf_.t[:, :], out_offset=None, in_=ckr[:, :],
                                                                        in_offset=bass.IndirectOffsetOnAxis(ap=off.t[:, bl * 64 + j:bl * 64 + j + 1], axis=0)),
                      [off], [rf_])
                    P.ops[-1]['dma'] = True
                    A(lambda e, j=j, lf_=lf_: e.copy(Kb.t[:, j, :], lf_.t[:, :]), [lf_], [(Kb, j)])
                    V(lambda e, k=k, rf_=rf_: e.tensor_copy(Kx.t[:, k, 64:96], rf_.t[:, :]), [rf_], [Kx])
                pa = nb()
                for k in range(4):
                    j = g * 4 + k
                    for cc in range(2):
                        T(lambda e, k=k, j=j, cc=cc, pa=pa: e.transpose(pa.t[:, (cc * 4 + k) * 128:(cc * 4 + k + 1) * 128], Kb.t[:, j, cc * 128:(cc + 1) * 128], ident.t[:, :]),
                          [(Kb, j), ident], [pa])
                V(lambda e, pa=pa: e.tensor_copy(KTc.t[:, :, :], pa.t[:, :].rearrange("p (c t) -> p c t", c=2)), [pa], [KTc])
                pr_ = nb()
                for k in range(4):
                    T(lambda e, k=k, pr_=pr_: e.transpose(pr_.t[0:96, k * 128:(k + 1) * 128], Kx.t[:, k, :], ident.t[:, :]), [Kx, ident], [pr_])
                A(lambda e, pr_=pr_: e.copy(KrT.t[64:96, :], pr_.t[64:96, 0:512]), [pr_], [KrT])
                pz = nf()
                for cc in range(2):
                    T(lambda e, cc=cc, pz=pz, qsl=qsl: e.matmul(pz.t[0:32, :], QlatT.t[:, cc, :, qsl], KTc.t[:, cc, :], start=(cc == 0), stop=False),
                      [QlatT, KTc], [pz])
                T(lambda e, pz=pz, qsl=qsl: e.matmul(pz.t[0:32, :], QTs.t[64:96, :, qsl], KrT.t[64:96, :], start=False, stop=True), [QTs, KrT], [pz])
                V(lambda e, pz=pz, g=g: e.tensor_copy(S_sb.t[:, g * 512:(g + 1) * 512], pz.t[0:32, :]), [pz], [S_sb])
            pz = nf()
            for cc in range(2):
                T(lambda e, cc=cc, pz=pz, qsl=qsl: e.matmul(pz.t[0:32, 0:4], QlatT.t[:, cc, :, qsl], kvT_s.t[:, cc, qsl], start=(cc == 0), stop=False),
                  [QlatT, kvT_s], [pz])
            T(lambda e, pz=pz, qsl=qsl: e.matmul(pz.t[0:32, 0:4], QTs.t[64:96, :, qsl], KnT.t[64:96, qsl], start=False, stop=True), [QTs, KnT], [pz])
            V(lambda e, pz=pz: e.tensor_tensor(S_sb.t[:, 8192:8196], pz.t[0:32, 0:4], m4f.t[:, 4:8], ALU.add), [pz, m4f], [S_sb])
            V(lambda e: e.tensor_reduce(smx3.t[:, :], S_sb.t[:, 0:8196], AX.X, ALU.max), [S_sb], [smx3])
            V(lambda e: e.tensor_scalar(smx3.t[:, :], smx3.t[:, :], -MLA_SCALE, None, ALU.mult), [smx3], [smx3])
            A(lambda e: e.activation(Pb16.t[:, 0:8196], S_sb.t[:, 0:8196], AF.Exp, bias=smx3.t[:, 0:1], scale=MLA_SCALE, accum_out=ssum3.t[:, 0:1]),
              [S_sb, smx3], [Pb16, ssum3])
            V(lambda e: e.reciprocal(ssum3.t[:, :], ssum3.t[:, :]), [ssum3], [ssum3])
            for half in range(2):
                pt = nb()
                for k in range(32):
                    j = half * 32 + k
                    T(lambda e, k=k, j=j, pt=pt: e.transpose(pt.t[:, k * 32:(k + 1) * 32], Pb16.t[:, j * 128:(j + 1) * 128], ident.t[0:32, 0:32]),
                      [Pb16, ident], [pt])
                V(lambda e, pt=pt, half=half: e.tensor_copy(PTk.t[:, half * 32:(half + 1) * 32, :], pt.t[:, :].rearrange("p (j r) -> p j r", j=32)),
                  [pt], [PTk])
            pt = nb()
            T(lambda e, pt=pt: e.transpose(pt.t[0:4, 0:32], Pb16.t[:, 8192:8196], ident.t[0:32, 0:32]), [Pb16, ident], [pt])
            V(lambda e, pt=pt: e.tensor_copy(PTk.t[0:4, 64, :], pt.t[0:4, 0:32]), [pt], [PTk])
            P.dma('sp', kvn_f.t[:, :], kvl_o[2048 + bl * 4:2048 + bl * 4 + 4, :], ['kvl_o'], [kvn_f])
            A(lambda e: e.copy(kvn_b.t[:, :], kvn_f.t[:, :]), [kvn_f], [kvn_b])
            po = nf()
            for j in range(64):
                T(lambda e, j=j, po=po: e.matmul(po.t[0:32, 0:256], PTk.t[:, j, :], Kb.t[:, j, :], start=(j == 0), stop=False), [PTk, (Kb, j)], [po])
            T(lambda e, po=po: e.matmul(po.t[0:32, 0:256], PTk.t[0:4, 64, :], kvn_b.t[:, :], start=False, stop=True), [PTk, kvn_b], [po])
            V(lambda e, po=po: e.tensor_scalar(O_sb.t[:, :], po.t[0:32, 0:256], ssum3.t[:, 0:1], None, ALU.mult), [po, ssum3], [O_sb])
            pt = nb()
            for cc in range(2):
                T(lambda e, cc=cc, pt=pt: e.transpose(pt.t[:, cc * 32:(cc + 1) * 32], O_sb.t[:, cc * 128:(cc + 1) * 128], ident.t[0:32, 0:32]), [O_sb, ident], [pt])
            V(lambda e, pt=pt: e.tensor_copy(OTb.t[:, :, :], pt.t[:, 0:64].rearrange("p (c r) -> p c r", c=2)), [pt], [OTb])
            for h in range(8):
                for cc in range(2):
                    T(lambda e, h=h, cc=cc, bl=bl: e.matmul(pm_view[0:64, h, bl * 4:(bl + 1) * 4], w_uv2.t[:, cc, h * 64:(h + 1) * 64], OTb.t[:, cc, h * 4:(h + 1) * 4],
                                                          start=(cc == 0), stop=(cc == 1)), [w_uv2, OTb], [psS])
        mlaTb = P.sb("mlaTb", [64, 8, 64], BF16)
        mla_s = P.sb("mla_s", [64, 512], F32)
        A(lambda e: e.copy(mlaTb.t[:, :, :], pm_view[0:64, :, :]), [psS], [mlaTb])
        pt = nb()
        for h in range(8):
            T(lambda e, h=h, pt=pt: e.transpose(pt.t[0:64, h * 64:(h + 1) * 64], mlaTb.t[:, h, :], ident.t[0:64, 0:64]), [mlaTb, ident], [pt])
        V(lambda e, pt=pt: e.tensor_copy(mla_s.t[:, :], pt.t[0:64, 0:512]), [pt], [mla_s])
        P.dma('sp', MIX[2048:2112, 0:512], mla_s.t[:, :], [mla_s], [('MIXa', 16)])
        P.release(p3)

    memkT_p = P.sb("memkT_p", [128, 8, 256], BF16)
    memv_b = P.sb("memv_b", [128, 2, D], BF16)
    mk0 = P.mark()
    mkb16 = P.sb("mkb16", [128, D], BF16)
    w_mk_sb = P.sb("w_mk_sb", [128, 8, D], BF16)
    w_mv_sb = P.sb("w_mv_sb", [128, 8, D], BF16)
    P.dma('pool', w_mk_sb.t[:, :, :], w_mk.rearrange("(c p) n -> p c n", p=128), (), [w_mk_sb])
    P.dma('pool', w_mv_sb.t[:, :, :], w_mv.rearrange("(c p) n -> p c n", p=128), (), [w_mv_sb])
    mem_f = [P.sb(f"mem_f{i}", [128, D], F32) for i in range(2)]
    mem_b = P.sb("mem_b", [128, D], BF16)
    memT = P.sb("memT", [128, 8, 128], BF16)
    mo_t = [P.sb(f"mo_t{i}", [128, D], F32) for i in range(2)]
    for mt in range(2):
        mf = mem_f[mt]
        P.dma('sp', mf.t[:, :], memp[mt * 128:(mt + 1) * 128, :], (), [mf])
        A(lambda e, mf=mf: e.copy(mem_b.t[:, :], mf.t[:, :]), [mf], [mem_b])
        pb = transposes(None, None, lambda i: mem_b.t[:, i * 128:(i + 1) * 128], mem_b, 8)
        V(lambda e, pb=pb: e.tensor_copy(memT.t[:, :, :], pb.t[:, :].rearrange("p (c t) -> p c t", c=8)), [pb], [memT])
        for wi, (wsb, o_ap) in enumerate(((w_mk_sb, mkp_o), (w_mv_sb, mvp_o))):
            ot = mo_t[wi]
            for n0 in (0, 512):
                pz = nf()
                for c in range(8):
                    T(lambda e, c=c, pz=pz, n0=n0, wsb=wsb: e.matmul(pz.t[:, :], memT.t[:, c, :], wsb.t[:, c, n0:n0 + 512],
                                                                   start=(c == 0), stop=(c == 7)), [memT, wsb], [pz])
                V(lambda e, pz=pz, n0=n0, ot=ot: e.tensor_copy(ot.t[:, n0:n0 + 512], pz.t[:, :]), [pz], [ot])
            P.dma('sp', o_ap[mt * 128:(mt + 1) * 128, :], ot.t[:, :], [ot], [f'memo{wi}'])
            if wi == 0:
                A(lambda e, ot=ot: e.copy(mkb16.t[:, :], ot.t[:, :]), [ot], [mkb16])
                pbm = transposes(None, None, lambda i: mkb16.t[:, i * 128:(i + 1) * 128], mkb16, 8)
                V(lambda e, pbm=pbm, mt=mt: e.tensor_copy(memkT_p.t[:, :, mt * 128:(mt + 1) * 128], pbm.t[:, :].rearrange("p (c t) -> p c t", c=8)),
                  [pbm], [memkT_p])
            else:
                A(lambda e, ot=ot, mt=mt: e.copy(memv_b.t[:, mt, :], ot.t[:, :]), [ot], [memv_b])
    P.release(mk0)

    ml0 = P.mark()
    ones_b = P.sb("ones_b", [128, 128], BF16)
    G(lambda e: e.memset(ones_b.t[:, :], 1.0), (), [ones_b])
    mlc = P.sb("mlc", [64, 2056], F32)
    lfh = P.sb("lfh", [64, 2, 4], BF16)
    lft = P.sb("lft", [64, 4], F32)
    gmb = P.sb("gmb", [64, 4], F32)
    dg = P.sb("dg", [64, 2, 4, 64], BF16)
    dgf = P.sb("dgf", [64, 4, 64], F32)
    dgr = P.sb("dgr", [64, 4, 64], F32)
    identf = P.sb("identf", [64, 64], F32)
    V(lambda e: e.tensor_copy(identf.t[:, :], ident.t[0:64, 0:64]), [ident], [identf])
    bt = P.sb("bt", [64, 4], F32)
    bend = P.sb("bend", [128, 4], F32)
    dec = P.sb("dec", [128, 4, 64], F32)
    decmax = P.sb("decmax", [128, 4], F32)
    m_rep = P.sb("m_rep", [128, 4], F32)
    m_new = P.sb("m_new", [128, 4], F32)
    a_prev = P.sb("a_prev", [128, 4], F32)
    wcol = P.sb("wcol", [64, 4], F32)
    kw = P.sb("kw", [64, 4, 128], BF16)
    v1 = P.sb("v1", [64, 4, 129], BF16)
    Cn = [P.sb(f"Cn{h}", [128, 129], F32) for h in range(4)]
    tri = P.sb("tri", [64, 64], BF16)
    G(lambda e: e.memset(tri.t[:, :], 1.0), (), [tri])
    G(lambda e: e.affine_select(tri.t[:, :], tri.t[:, :], [[1, 64]], ALU.is_ge, 0.0, base=0, channel_multiplier=-1), [tri], [tri])

    Dm = P.sb("Dm", [64, 4, 64], F32)
    Rsb = P.sb("Rsb", [128, 4, 64], F32)
    dmax = P.sb("dmax", [64, 4], F32)
    inter = P.sb("inter", [64, 4], F32)
    mtt = P.sb("mtt", [64, 4], F32)
    winter = P.sb("winter", [64, 4], F32)
    emt = P.sb("emt", [64, 4], F32)
    qk16 = P.sb("qk16", [64, 8, 128], BF16)
    qkT = P.sb("qkT", [128, 8, 64], BF16)
    Ab = P.sb("Ab", [64, 4, 64], BF16)
    ATb = P.sb("ATb", [64, 4, 64], BF16)
    Cnb = [P.sb(f"Cnb{h}", [128, 129], BF16) for h in range(4)]
    av = P.sb("av", [64, 129], F32)
    nd = P.sb("nd", [64, 129], F32)
    dd = P.sb("dd", [64, 1], F32)
    mlo = P.sb("mlo", [64, 512], F32)

    def mlstm_chunk(row0, L):
        P.dma('sp', mlc.t[0:L, :], ML[row0:row0 + L, :], [('ML', row0 // 128)], [mlc])
        V(lambda e: e.tensor_copy(lfh.t[0:L, 0, :], mlc.t[0:L, 2052:2056]), [mlc], [lfh])
        V(lambda e: e.tensor_copy(lft.t[0:L, :], lfh.t[0:L, 0, :]), [lfh], [lft])
        V(lambda e: e.tensor_tensor(lft.t[0:L, :], mlc.t[0:L, 2052:2056], lft.t[0:L, :], ALU.subtract), [mlc, lft], [lft])
        V(lambda e: e.tensor_copy(lfh.t[0:L, 1, :], lft.t[0:L, :]), [lft], [lfh])
        pc = nf()
        for i in range(2):
            T(lambda e, i=i: e.matmul(pc.t[0:L, 0:4], tri.t[0:L, 0:L], lfh.t[0:L, i, :], start=(i == 0), stop=(i == 1)), [tri, lfh], [pc])
        for i in range(2):
            T(lambda e, i=i: e.matmul(pc.t[:, 4:8], ones_b.t[0:L, :], lfh.t[0:L, i, :], start=(i == 0), stop=(i == 1)), [ones_b, lfh], [pc])
        V(lambda e: e.tensor_copy(bt.t[0:L, :], pc.t[0:L, 0:4]), [pc], [bt])
        V(lambda e: e.tensor_copy(bend.t[:, :], pc.t[:, 4:8]), [pc], [bend])
        V(lambda e: e.tensor_tensor(gmb.t[0:L, :], mlc.t[0:L, 2048:2052], bt.t[0:L, :], ALU.subtract), [mlc, bt], [gmb])
        V(lambda e: e.tensor_tensor(dgf.t[0:L, :, 0:L], identf.t[0:L, 0:L].unsqueeze(1).broadcast_to([L, 4, L]),
                                    gmb.t[0:L, :].unsqueeze(2).broadcast_to([L, 4, L]), ALU.mult), [identf, gmb], [dgf])
        V(lambda e: e.tensor_copy(dg.t[0:L, 0, :, 0:L], dgf.t[0:L, :, 0:L]), [dgf], [dg])
        V(lambda e: e.tensor_copy(dgr.t[0:L, :, 0:L], dg.t[0:L, 0, :, 0:L]), [dg], [dgr])
        V(lambda e: e.tensor_tensor(dgr.t[0:L, :, 0:L], dgf.t[0:L, :, 0:L], dgr.t[0:L, :, 0:L], ALU.subtract), [dgf, dgr], [dgr])
        V(lambda e: e.tensor_copy(dg.t[0:L, 1, :, 0:L], dgr.t[0:L, :, 0:L]), [dgr], [dg])
        pr = nf()
        for h in range(4):
            for i in range(2):
                T(lambda e, i=i, h=h: e.matmul(pr.t[:, h * 64:h * 64 + L], ones_b.t[0:L, :], dg.t[0:L, i, h, 0:L],
                                               start=(i == 0), stop=(i == 1)), [ones_b, dg], [pr])
        V(lambda e, pr=pr: e.tensor_copy(Rsb.t[:, :, 0:L], pr.t[:, 0:256].rearrange("p (h s) -> p h s", h=4)[:, :, 0:L]), [pr], [Rsb])
        prv = Rsb.t[:, :, :]
        A(lambda e: e.copy(v1.t[0:L, :, 0:128], mlc.t[0:L, 1024:1536].rearrange("p (h d) -> p h d", h=4)), [mlc], [v1])
        V(lambda e: e.tensor_tensor(Dm.t[0:L, :, 0:L], prv[0:L, :, 0:L], bt.t[0:L, :].unsqueeze(2).broadcast_to([L, 4, L]), ALU.add),
          [Rsb, bt], [Dm])
        V(lambda e: e.tensor_tensor(Dm.t[0:L, :, 0:L], Dm.t[0:L, :, 0:L], maskc.t[0:L, 0:L].unsqueeze(1).broadcast_to([L, 4, L]), ALU.add),
          [Dm, maskc], [Dm])
        V(lambda e: e.tensor_reduce(dmax.t[0:L, :], Dm.t[0:L, :, 0:L], AX.X, ALU.max), [Dm], [dmax])
        V(lambda e: e.tensor_tensor(inter.t[0:L, :], bt.t[0:L, :], m_rep.t[0:L, :], ALU.add), [bt, m_rep], [inter])
        V(lambda e: e.tensor_tensor(mtt.t[0:L, :], inter.t[0:L, :], dmax.t[0:L, :], ALU.max), [inter, dmax], [mtt])
        V(lambda e: e.tensor_tensor(Dm.t[0:L, :, 0:L], Dm.t[0:L, :, 0:L], mtt.t[0:L, :].unsqueeze(2).broadcast_to([L, 4, L]), ALU.subtract),
          [Dm, mtt], [Dm])
        A(lambda e: e.activation(Dm.t[0:L, :, 0:L], Dm.t[0:L, :, 0:L], AF.Exp), [Dm], [Dm])
        V(lambda e: e.tensor_tensor(winter.t[0:L, :], inter.t[0:L, :], mtt.t[0:L, :], ALU.subtract), [inter, mtt], [winter])
        A(lambda e: e.activation(winter.t[0:L, :], winter.t[0:L, :], AF.Exp), [winter], [winter])
        A(lambda e: e.activation(emt.t[0:L, :], mtt.t[0:L, :], AF.Exp, scale=-1.0), [mtt], [emt])
        A(lambda e: e.copy(qk16.t[0:L, :, :], mlc.t[0:L, 0:1024].rearrange("p (h d) -> p h d", h=8)), [mlc], [qk16])
        pbk = nb()
        for j in range(8):
            T(lambda e, j=j, pbk=pbk: e.transpose(pbk.t[:, j * 64:j * 64 + L], qk16.t[0:L, j, :], ident.t[0:L, 0:L]), [qk16, ident], [pbk])
        V(lambda e, pbk=pbk: e.tensor_copy(qkT.t[:, :, 0:L], pbk.t[:, 0:512].rearrange("p (j t) -> p j t", j=8)[:, :, 0:L]), [pbk], [qkT])
        pq = nf()
        for h in range(4):
            T(lambda e, h=h, pq=pq: e.matmul(pq.t[0:L, h * 64:h * 64 + L], qkT.t[:, h, 0:L], qkT.t[:, 4 + h, 0:L], start=True, stop=True),
              [qkT], [pq])
        V(lambda e, pq=pq: e.tensor_tensor(Ab.t[0:L, :, 0:L], Dm.t[0:L, :, 0:L], pq.t[:, 0:256].rearrange("p (h s) -> p h s", h=4)[0:L, :, 0:L], ALU.mult),
          [Dm, pq], [Ab])
        pbk2 = nb()
        for h in range(4):
            T(lambda e, h=h, pbk2=pbk2: e.transpose(pbk2.t[0:L, h * 64:h * 64 + L], Ab.t[0:L, h, 0:L], ident.t[0:L, 0:L]), [Ab, ident], [pbk2])
        V(lambda e, pbk2=pbk2: e.tensor_copy(ATb.t[0:L, :, 0:L], pbk2.t[:, 0:256].rearrange("p (h t) -> p h t", h=4)[0:L, :, 0:L]), [pbk2], [ATb])
        for h in range(4):
            A(lambda e, h=h: e.copy(Cnb[h].t[:, :], Cn[h].t[:, :]), [Cn[h]], [Cnb[h]])
            pn1 = nf()
            T(lambda e, h=h, pn1=pn1: e.matmul(pn1.t[0:L, 0:129], qkT.t[:, h, 0:L], Cnb[h].t[:, :], start=True, stop=True), [qkT, Cnb[h]], [pn1])
            pn2 = nf()
            T(lambda e, h=h, pn2=pn2: e.matmul(pn2.t[0:L, 0:129], ATb.t[0:L, h, 0:L], v1.t[0:L, h, :], start=True, stop=True), [ATb, v1], [pn2])
            A(lambda e, pn2=pn2: e.copy(av.t[0:L, :], pn2.t[0:L, 0:129]), [pn2], [av])
            V(lambda e, h=h, pn1=pn1: e.scalar_tensor_tensor(nd.t[0:L, :], pn1.t[0:L, 0:129], winter.t[0:L, h:h + 1], av.t[0:L, :], ALU.mult, ALU.add),
              [pn1, winter, av], [nd])
            A(lambda e: e.activation(dd.t[0:L, :], nd.t[0:L, 128:129], AF.Abs), [nd], [dd])
            V(lambda e, h=h: e.tensor_tensor(dd.t[0:L, :], dd.t[0:L, :], emt.t[0:L, h:h + 1], ALU.max), [dd, emt], [dd])
            V(lambda e: e.reciprocal(dd.t[0:L, :], dd.t[0:L, :]), [dd], [dd])
            V(lambda e, h=h: e.scalar_tensor_tensor(mlo.t[0:L, h * 128:(h + 1) * 128], nd.t[0:L, 0:128], dd.t[0:L, 0:1],
                                                    mlc.t[0:L, 1536 + h * 128:1536 + (h + 1) * 128], ALU.mult, ALU.mult), [nd, dd, mlc], [mlo])
        P.dma('sp', MIX[row0:row0 + L, 512:1024], mlo.t[0:L, :], [mlo], [('MIXb', row0 // 128)])
        V(lambda e: e.tensor_tensor(dec.t[:, :, 0:L], prv[:, :, 0:L],
                                    bend.t[:, :].unsqueeze(2).broadcast_to([128, 4, L]), ALU.add), [Rsb, bend], [dec])
        V(lambda e: e.tensor_reduce(decmax.t[:, :], dec.t[:, :, 0:L], AX.X, ALU.max), [dec], [decmax])
        V(lambda e: e.tensor_tensor(m_new.t[:, :], bend.t[:, :], m_rep.t[:, :], ALU.add), [bend, m_rep], [m_new])
        V(lambda e: e.tensor_tensor(a_prev.t[:, :], m_new.t[:, :], decmax.t[:, :], ALU.max), [m_new, decmax], [a_prev])
        V(lambda e: e.tensor_tensor(m_new.t[:, :], m_new.t[:, :], a_prev.t[:, :], ALU.subtract), [m_new, a_prev], [m_new])
        V(lambda e: e.tensor_copy(m_rep.t[:, :], a_prev.t[:, :]), [a_prev], [m_rep])
        A(lambda e: e.activation(a_prev.t[:, :], m_new.t[:, :], AF.Exp), [m_new], [a_prev])
        V(lambda e: e.tensor_tensor(wcol.t[0:L, :], gmb.t[0:L, :], bend.t[0:L, :], ALU.add), [gmb, bend], [wcol])
        V(lambda e: e.tensor_tensor(wcol.t[0:L, :], wcol.t[0:L, :], m_rep.t[0:L, :], ALU.subtract), [wcol, m_rep], [wcol])
        A(lambda e: e.activation(wcol.t[0:L, :], wcol.t[0:L, :], AF.Exp), [wcol], [wcol])
        V(lambda e: e.tensor_tensor(kw.t[0:L, :, :], mlc.t[0:L, 512:1024].rearrange("p (h d) -> p h d", h=4),
                                    wcol.t[0:L, :].unsqueeze(2).broadcast_to([L, 4, 128]), ALU.mult), [mlc, wcol], [kw])
        for h in range(4):
            pu = nf()
            T(lambda e, h=h, pu=pu: e.matmul(pu.t[:, 0:129], kw.t[0:L, h, :], v1.t[0:L, h, :], start=True, stop=True), [kw, v1], [pu])
            V(lambda e, h=h, pu=pu: e.scalar_tensor_tensor(Cn[h].t[:, :], Cn[h].t[:, :], a_prev.t[:, h:h + 1], pu.t[:, 0:129],
                                                           ALU.mult, ALU.add), [Cn[h], a_prev, pu], [Cn[h]])

    G(lambda e: e.memset(v1.t[:, :, 128:129], 1.0), (), [v1])
    for h in range(4):
        G(lambda e, h=h: e.memset(Cn[h].t[:, :], 0.0), (), [Cn[h]])
    G(lambda e: e.memset(m_rep.t[:, :], 0.0), (), [m_rep])
    for ck in range(32):
        mlstm_chunk(ck * 64, 64)
    for h in range(4):
        P.dma('sp', Cp_o[h, :, :], Cn[h].t[:, 0:128], [Cn[h]], ['Cp_o'])
        P.dma('sp', np_o[h:h + 1, :].rearrange("o d -> d o"), Cn[h].t[:, 128:129], [Cn[h]], ['np_o'])
    P.dma('sp', mp_o[0:1, :], m_rep.t[0:1, :], [m_rep], ['mp_o'])
    for bl in range(16):
        for h in range(4):
            P.dma('sp', Cn[h].t[:, 0:128], stC[bl, h, :, :], (), [Cn[h]])
            P.dma('sp', Cn[h].t[:, 128:129], stn[bl, h:h + 1, :].rearrange("o d -> d o"), (), [Cn[h]])
        P.dma('sp', m_rep.t[:, :], stm[bl, :].partition_broadcast(128), (), [m_rep])
        mlstm_chunk(2048 + bl * 4, 4)
        for h in range(4):
            P.dma('sp', Cs_o[bl, h, :, :], Cn[h].t[:, 0:128], [Cn[h]], ['Cs_o'])
            P.dma('sp', ns_o[bl, h:h + 1, :].rearrange("o d -> d o"), Cn[h].t[:, 128:129], [Cn[h]], ['ns_o'])
        P.dma('sp', ms_o[bl:bl + 1, :], m_rep.t[0:1, :], [m_rep], ['ms_o'])
    P.release(ml0)
    f0 = P.mark()
    wsb = {}
    for nm, wd in (("w_out", w_out), ("w_mq", w_mq), ("w_mo", w_mo), ("w_pq", w_pq)):
        wsb[nm] = P.sb(nm + "_sb", [128, 8, D], BF16)
        P.dma('pool', wsb[nm].t[:, :, :], wd.rearrange("(c p) n -> p c n", p=128), (), [wsb[nm]])
    lng = [None] + [P.sb(f"lng{i}", [128, D], F32) for i in (1, 2, 3)]
    lnb = [None] + [P.sb(f"lnb{i}", [128, D], F32) for i in (1, 2, 3)]
    for i in (1, 2, 3):
        P.dma('sp', lng[i].t[:, :], ln_g[i].partition_broadcast(128), (), [lng[i]])
        P.dma('sp', lnb[i].t[:, :], ln_b[i].partition_broadcast(128), (), [lnb[i]])
    k12 = P.sb("k12", [128, 128], BF16)
    P.dma('pool', k12.t[0:64, :], k1T, (), [k12])
    P.dma('pool', k12.t[64:128, :], k2T, (), [k12])
    io16i = P.sb("io16i", [128, 16], I32)
    io16 = P.sb("io16", [128, 16], F32)
    G(lambda e: e.iota(io16i.t[:, :], [[1, 16]], base=0, channel_multiplier=0), (), [io16i])
    V(lambda e: e.tensor_copy(io16.t[:, :], io16i.t[:, :]), [io16i], [io16])
    zt = P.sb("zt", [128, 512], F32)
    G(lambda e: e.memset(zt.t[:, :], 0.0), (), [zt])
    if not _PH3:
        P.dma('sp', MIX[2048:2176, 0:512], zt.t[:, :], [zt], [('MIXa', 16)])

    NG = 4
    Ug = [P.sb(f"Ug{i}", [128, D], F32) for i in range(NG)]
    mixf = Ug[0]
    xb2 = P.sb("xb2", [128, D], BF16)
    tT = P.sb("tT", [128, 8, 128], BF16)
    xn_t = Ug[1]
    x1 = P.sb("x1", [128, D], F32)
    x2 = P.sb("x2", [128, D], F32)
    yac = x1
    qmT = P.sb("qmT", [128, 8, 128], BF16)
    Pm = P.sb("Pm", [128, 4, 256], BF16)
    PmT = P.sb("PmT", [128, 8, 128], BF16)
    oT = P.sb("oT", [128, 8, 128], BF16)
    smx = P.sb("smx", [128, 4], F32)
    ssum = P.sb("ssum", [128, 4], F32)
    s12 = P.sb("s12", [128, 2, 8, 128], F32)
    w128 = P.sb("w128", [128, 128], F32)
    w256 = P.sb("w256", [128, 256], F32)
    v12 = P.sb("v12", [128, 2, 8, 16], F32)
    i12 = P.sb("i12", [128, 2, 8, 16], U32)
    i12f = P.sb("i12f", [128, 2, 8, 16], F32)
    cand = P.sb("cand", [128, 8, 256], F32)
    sc = P.sb("sc", [128, 8, 16], F32)
    jx = P.sb("jx", [128, 8, 16], U32)
    k1i = P.sb("k1i", [128, 8, 16], U32)
    k2i = P.sb("k2i", [128, 8, 16], U32)
    k12f = P.sb("k12f", [128, 2, 8, 16], F32)
    oh = P.sb("oh", [128, 8, 16, 16], F32)
    isel = P.sb("isel", [128, 2, 8, 16], F32)
    ef = P.sb("ef", [128, 128], F32)
    ei = P.sb("ei", [128, 128], I32)
    gat = P.sb("gat", [128, 8, 16], F32)
    hd = P.sb("hd", [128, 128], F32)
    aa = P.sb("aa", [128, 128], F32)

    def to_T(src, dst):
        A(lambda e: e.copy(xb2.t[:, :], src.t[:, :]), [src], [xb2])
        pbk = transposes(None, None, lambda i: xb2.t[:, i * 128:(i + 1) * 128], xb2, 8)
        V(lambda e, pbk=pbk: e.tensor_copy(dst.t[:, :, :], pbk.t[:, :].rearrange("p (c t) -> p c t", c=8)), [pbk], [dst])

    def proj_T(wt, srcT, dst):
        for half in range(2):
            pz = nf()
            for j in range(4):
                ec = half * 4 + j
                for dc in range(8):
                    T(lambda e, j=j, ec=ec, dc=dc, pz=pz: e.matmul(pz.t[:, j * 128:(j + 1) * 128], wt.t[:, dc, ec * 128:(ec + 1) * 128], srcT.t[:, dc, :],
                                                                 start=(dc == 0), stop=(dc == 7)), [wt, srcT], [pz])
            A(lambda e, half=half, pz=pz: e.copy(dst.t[:, half * 4:(half + 1) * 4, :], pz.t[:, :].rearrange("p (c t) -> p c t", c=4)), [pz], [dst])

    def proj_res(wt, srcT, res, dst):
        for n0 in (0, 512):
            pz = nf()
            for dc in range(8):
                T(lambda e, dc=dc, pz=pz, n0=n0: e.matmul(pz.t[:, :], srcT.t[:, dc, :], wt.t[:, dc, n0:n0 + 512], start=(dc == 0), stop=(dc == 7)),
                  [wt, srcT], [pz])
            V(lambda e, pz=pz, n0=n0: e.scalar_tensor_tensor(dst.t[:, n0:n0 + 512], res.t[:, n0:n0 + 512], ALPHA, pz.t[:, :], ALU.mult, ALU.add),
              [res, pz], [dst])

    def top16(vals_ap, idx_ap, src_ap, work_ap, bufs_r, bufs_w):
        V(lambda e: e.max(vals_ap[:, 0:8], src_ap), bufs_r, bufs_w)
        V(lambda e: e.max_index(idx_ap[:, 0:8], vals_ap[:, 0:8], src_ap), bufs_r + bufs_w, bufs_w)
        V(lambda e: e.match_replace(work_ap, vals_ap[:, 0:8], src_ap, NEG), bufs_r + bufs_w, bufs_w)
        V(lambda e: e.max(vals_ap[:, 8:16], work_ap), bufs_w, bufs_w)
        V(lambda e: e.max_index(idx_ap[:, 8:16], vals_ap[:, 8:16], work_ap), bufs_w, bufs_w)

    cmk_b = P.sb("cmk_b", [128, 2, D], BF16)
    cmv_b = P.sb("cmv_b", [128, 2, D], BF16)
    cmkT = P.sb("cmkT", [128, 8, 256], BF16)
    PmT4 = P.sb("PmT4", [128, 8, 4], BF16)

    def sample_mem_attn(qmT, oT):
        for bl in range(16):
            kf_ap = s12.t[:, :, :, :].rearrange("p a h n -> p a (h n)")
            vf_ap = cand.t[:, :, :].rearrange("p (a h) n -> p a (h n)", a=2)
            P.dma('sp', kf_ap, cmk[bl].rearrange("(c p) n -> p c n", p=128), (), [s12])
            P.dma('sp', vf_ap, cmv[bl].rearrange("(c p) n -> p c n", p=128), (), [cand])
            A(lambda e, kf_ap=kf_ap: e.copy(cmk_b.t[:, :, :], kf_ap), [s12], [cmk_b])
            V(lambda e, vf_ap=vf_ap: e.tensor_copy(cmv_b.t[:, :, :], vf_ap), [cand], [cmv_b])
            for mt in range(2):
                pbk = transposes(None, None, lambda i, mt=mt: cmk_b.t[:, mt, i * 128:(i + 1) * 128], cmk_b, 8)
                V(lambda e, pbk=pbk, mt=mt: e.tensor_copy(cmkT.t[:, :, mt * 128:(mt + 1) * 128], pbk.t[:, :].rearrange("p (c t) -> p c t", c=8)),
                  [pbk], [cmkT])
            for hp in range(2):
                pz = nf()
                for hh in range(2):
                    h = hp * 2 + hh
                    for c in range(2):
                        T(lambda e, hh=hh, h=h, c=c, pz=pz, bl=bl: e.matmul(pz.t[0:4, hh * 256:(hh + 1) * 256], qmT.t[:, 2 * h + c, bl * 4:(bl + 1) * 4],
                                                                          cmkT.t[:, 2 * h + c, :], start=(c == 0), stop=(c == 1)), [qmT, cmkT], [pz])
                V(lambda e, hp=hp, pz=pz: e.tensor_reduce(smx.t[0:4, hp * 2:hp * 2 + 2], pz.t[0:4, :].rearrange("p (h m) -> p h m", h=2), AX.X, ALU.max),
                  [pz], [smx])
                V(lambda e, hp=hp: e.tensor_scalar(smx.t[0:4, hp * 2:hp * 2 + 2], smx.t[0:4, hp * 2:hp * 2 + 2], -1.0 / 16, None, ALU.mult), [smx], [smx])
                for hh in range(2):
                    h = hp * 2 + hh
                    A(lambda e, hh=hh, h=h, pz=pz: e.activation(Pm.t[0:4, h, :], pz.t[0:4, hh * 256:(hh + 1) * 256], AF.Exp, bias=smx.t[0:4, h:h + 1],
                                                              scale=1.0 / 16, accum_out=ssum.t[0:4, h:h + 1]), [pz, smx], [Pm, ssum])
            V(lambda e: e.reciprocal(ssum.t[0:4, :], ssum.t[0:4, :]), [ssum], [ssum])
            V(lambda e: e.tensor_tensor(Pm.t[0:4, :, :], Pm.t[0:4, :, :], ssum.t[0:4, :].unsqueeze(2).broadcast_to([4, 4, 256]), ALU.mult), [Pm, ssum], [Pm])
            pbk = nb()
            for i in range(8):
                T(lambda e, i=i, pbk=pbk: e.transpose(pbk.t[:, i * 128:i * 128 + 4], Pm.t[0:4, i // 2, (i % 2) * 128:(i % 2 + 1) * 128], ident.t[0:4, 0:4]),
                  [Pm, ident], [pbk])
            V(lambda e, pbk=pbk: e.tensor_copy(PmT4.t[:, :, :], pbk.t[:, :].rearrange("p (c t) -> p c t", c=8)[:, :, 0:4]), [pbk], [PmT4])
            for ec in range(8):
                h = ec // 2
                for mc in range(2):
                    T(lambda e, ec=ec, h=h, mc=mc, bl=bl: e.matmul(psS.t[:, ec * 64 + bl * 4:ec * 64 + bl * 4 + 4], cmv_b.t[:, mc, ec * 128:(ec + 1) * 128],
                                                                 PmT4.t[:, h * 2 + mc, :], start=(mc == 0), stop=(mc == 1)), [cmv_b, PmT4], [psS])
        A(lambda e: e.copy(oT.t[:, :, 0:64], psS.t[:, 0:512].rearrange("p (c t) -> p c t", c=8)), [psS], [oT])

    for tt in range(NT):
        r0 = tt * 128
        P.dma('sp', mixf.t[:, :], MIX[r0:r0 + 128, :], [('MIXa', tt), ('MIXb', tt)] + ([('MIXb', 16 + 0)] if tt == 16 else []), [mixf])
        P.dma('sp', xn_t.t[:, :], XN[r0:r0 + 128, :], [('XN', tt)], [xn_t])
        if tt == 16:
            G(lambda e: e.memset(mixf.t[64:128, :], 0.0), [mixf], [mixf])
        to_T(mixf, tT)
        proj_res(wsb["w_out"], tT, xn_t, x1)
        layernorm(x1, lng[1], lnb[1])
        P.dma('sp', X1[r0:r0 + 128, :], x1.t[:, :], [x1], [('X1', tt)])
        to_T(x1, tT)
        proj_T(wsb["w_mq"], tT, qmT)
        if tt < 16:
            for hp in range(2):
                pz = nf()
                for hh in range(2):
                    h = hp * 2 + hh
                    for c in range(2):
                        T(lambda e, hh=hh, h=h, c=c, pz=pz: e.matmul(pz.t[:, hh * 256:(hh + 1) * 256], qmT.t[:, 2 * h + c, :], memkT_p.t[:, 2 * h + c, :],
                                                                   start=(c == 0), stop=(c == 1)), [qmT, memkT_p], [pz])
                V(lambda e, hp=hp, pz=pz: e.tensor_reduce(smx.t[:, hp * 2:hp * 2 + 2], pz.t[:, :].rearrange("p (h m) -> p h m", h=2), AX.X, ALU.max),
                  [pz], [smx])
                V(lambda e, hp=hp: e.tensor_scalar(smx.t[:, hp * 2:hp * 2 + 2], smx.t[:, hp * 2:hp * 2 + 2], -1.0 / 16, None, ALU.mult), [smx], [smx])
                for hh in range(2):
                    h = hp * 2 + hh
                    A(lambda e, hh=hh, h=h, pz=pz: e.activation(Pm.t[:, h, :], pz.t[:, hh * 256:(hh + 1) * 256], AF.Exp, bias=smx.t[:, h:h + 1],
                                                              scale=1.0 / 16, accum_out=ssum.t[:, h:h + 1]), [pz, smx], [Pm, ssum])
            V(lambda e: e.reciprocal(ssum.t[:, :], ssum.t[:, :]), [ssum], [ssum])
            V(lambda e: e.tensor_tensor(Pm.t[:, :, :], Pm.t[:, :, :], ssum.t[:, :].unsqueeze(2).broadcast_to([128, 4, 256]), ALU.mult), [Pm, ssum], [Pm])
            pbk = transposes(None, None, lambda i: Pm.t[:, i // 2, (i % 2) * 128:(i % 2 + 1) * 128], Pm, 8)
            V(lambda e, pbk=pbk: e.tensor_copy(PmT.t[:, :, :], pbk.t[:, :].rearrange("p (c t) -> p c t", c=8)), [pbk], [PmT])
            for half in range(2):
                pz = nf()
                for j in range(4):
                    ec = half * 4 + j
                    h = ec // 2
                    for mc in range(2):
                        T(lambda e, j=j, ec=ec, h=h, mc=mc, pz=pz: e.matmul(pz.t[:, j * 128:(j + 1) * 128], memv_b.t[:, mc, ec * 128:(ec + 1) * 128],
                                                                         PmT.t[:, h * 2 + mc, :], start=(mc == 0), stop=(mc == 1)), [memv_b, PmT], [pz])
                A(lambda e, half=half, pz=pz: e.copy(oT.t[:, half * 4:(half + 1) * 4, :], pz.t[:, :].rearrange("p (c t) -> p c t", c=4)), [pz], [oT])
        else:
            sample_mem_attn(qmT, oT)
        proj_res(wsb["w_mo"], oT, x1, x2)
        layernorm(x2, lng[2], lnb[2])
        P.dma('sp', X2[r0:r0 + 128, :], x2.t[:, :], [x2], [('X2', tt)])
        to_T(x2, tT)
        proj_T(wsb["w_pq"], tT, qmT)
        for w12 in range(2):
            for half in range(2):
                pz = nf()
                for j in range(4):
                    h = half * 4 + j
                    T(lambda e, j=j, h=h, w12=w12, pz=pz: e.matmul(pz.t[:, j * 128:(j + 1) * 128], qmT.t[w12 * 64:(w12 + 1) * 64, h, :],
                                                                 k12.t[w12 * 64:(w12 + 1) * 64, :], start=True, stop=True), [qmT, k12], [pz])
                V(lambda e, half=half, w12=w12, pz=pz: e.tensor_copy(s12.t[:, w12, half * 4:(half + 1) * 4, :], pz.t[:, :].rearrange("p (c n) -> p c n", c=4)),
                  [pz], [s12])
        for w12 in range(2):
            for h in range(8):
                top16(v12.t[:, w12, h, :], i12.t[:, w12, h, :], s12.t[:, w12, h, :], w128.t[:, :], [s12], [v12, i12, w128])
        V(lambda e: e.tensor_tensor(cand.t[:, :, :].rearrange("p h (a b) -> p h a b", a=16), v12.t[:, 0, :, :].unsqueeze(3).broadcast_to([128, 8, 16, 16]),
                                    v12.t[:, 1, :, :].unsqueeze(2).broadcast_to([128, 8, 16, 16]), ALU.add), [v12], [cand])
        for h in range(8):
            top16(sc.t[:, h, :], jx.t[:, h, :], cand.t[:, h, :], w256.t[:, :], [cand], [sc, jx, w256])
        V(lambda e: e.tensor_tensor(gat.t[:, :, :], sc.t[:, :, :], sc.t[:, :, 0:1].broadcast_to([128, 8, 16]), ALU.subtract), [sc], [gat])
        A(lambda e: e.activation(gat.t[:, :, :], gat.t[:, :, :], AF.Exp), [gat], [gat])
        V(lambda e: e.tensor_reduce(smx.t[:, 0:4], gat.t[:, 0:4, :], AX.X, ALU.add), [gat], [smx])
        V(lambda e: e.tensor_reduce(ssum.t[:, 0:4], gat.t[:, 4:8, :], AX.X, ALU.add), [gat], [ssum])
        V(lambda e: e.reciprocal(smx.t[:, :], smx.t[:, :]), [smx], [smx])
        V(lambda e: e.reciprocal(ssum.t[:, :], ssum.t[:, :]), [ssum], [ssum])
        V(lambda e: e.tensor_tensor(gat.t[:, 0:4, :], gat.t[:, 0:4, :], smx.t[:, :].unsqueeze(2).broadcast_to([128, 4, 16]), ALU.mult), [gat, smx], [gat])
        V(lambda e: e.tensor_tensor(gat.t[:, 4:8, :], gat.t[:, 4:8, :], ssum.t[:, :].unsqueeze(2).broadcast_to([128, 4, 16]), ALU.mult), [gat, ssum], [gat])
        V(lambda e: e.tensor_single_scalar(k1i.t[:, :, :], jx.t[:, :, :], 4, ALU.logical_shift_right), [jx], [k1i])
        V(lambda e: e.tensor_single_scalar(k2i.t[:, :, :], jx.t[:, :, :], 15, ALU.bitwise_and), [jx], [k2i])
        V(lambda e: e.tensor_copy(k12f.t[:, 0, :, :], k1i.t[:, :, :]), [k1i], [k12f])
        V(lambda e: e.tensor_copy(k12f.t[:, 1, :, :], k2i.t[:, :, :]), [k2i], [k12f])
        V(lambda e: e.tensor_copy(i12f.t[:, :, :, :], i12.t[:, :, :, :]), [i12], [i12f])
        for w12 in range(2):
            V(lambda e, w12=w12: e.tensor_tensor(oh.t[:, :, :, :], k12f.t[:, w12, :, :].unsqueeze(3).broadcast_to([128, 8, 16, 16]),
                                                 io16.t[:, :].unsqueeze(1).unsqueeze(1).broadcast_to([128, 8, 16, 16]), ALU.is_equal), [k12f, io16], [oh])
            V(lambda e, w12=w12: e.tensor_tensor(oh.t[:, :, :, :], oh.t[:, :, :, :], i12f.t[:, w12, :, :].unsqueeze(2).broadcast_to([128, 8, 16, 16]), ALU.mult),
              [oh, i12f], [oh])
            V(lambda e, w12=w12: e.tensor_reduce(isel.t[:, w12, :, :], oh.t[:, :, :, :], AX.X, ALU.add), [oh], [isel])
        V(lambda e: e.scalar_tensor_tensor(ef.t[:, :], isel.t[:, 0, :, :].rearrange("p h k -> p (h k)"), 128.0,
                                           isel.t[:, 1, :, :].rearrange("p h k -> p (h k)"), ALU.mult, ALU.add), [isel], [ef])
        V(lambda e: e.tensor_scalar(ef.t[:, :], ef.t[:, :], 0.0, 16383.0, ALU.max, ALU.min), [ef], [ef])
        V(lambda e: e.tensor_copy(ei.t[:, :], ef.t[:, :]), [ef], [ei])
        for sl in range(128):
            ug = Ug[sl % NG]
            G(lambda e, sl=sl, ug=ug: e.indirect_dma_start(out=ug.t[:, :], out_offset=None, in_=peer_u[:, :],
                                                           in_offset=bass.IndirectOffsetOnAxis(ap=ei.t[:, sl:sl + 1], axis=0)), [ei], [ug])
            P.ops[-1]['dma'] = True
            V(lambda e, sl=sl, ug=ug: e.tensor_tensor(ug.t[:, :], ug.t[:, :], x2.t[:, :], ALU.mult), [ug, x2], [ug])
            V(lambda e, sl=sl, ug=ug: e.tensor_reduce(hd.t[:, sl:sl + 1], ug.t[:, :], AX.X, ALU.add), [ug], [(hd, sl)])
        A(lambda e: e.activation(aa.t[:, :], hd.t[:, :], AF.Gelu), [(hd, s_) for s_ in range(128)], [aa])
        V(lambda e: e.tensor_tensor(aa.t[:, :], aa.t[:, :], gat.t[:, :, :].rearrange("p h k -> p (h k)"), ALU.mult), [aa, gat], [aa])
        for sl in range(128):
            ug = Ug[sl % NG]
            G(lambda e, sl=sl, ug=ug: e.indirect_dma_start(out=ug.t[:, :], out_offset=None, in_=peer_v[:, :],
                                                           in_offset=bass.IndirectOffsetOnAxis(ap=ei.t[:, sl:sl + 1], axis=0)), [ei], [ug])
            P.ops[-1]['dma'] = True
            if sl == 0:
                V(lambda e, ug=ug: e.tensor_scalar(yac.t[:, :], ug.t[:, :], aa.t[:, 0:1], None, ALU.mult), [ug, aa], [yac])
            else:
                V(lambda e, sl=sl, ug=ug: e.scalar_tensor_tensor(yac.t[:, :], ug.t[:, :], aa.t[:, sl:sl + 1], yac.t[:, :], ALU.mult, ALU.add),
                  [ug, aa, yac], [yac])
        V(lambda e: e.scalar_tensor_tensor(yac.t[:, :], x2.t[:, :], ALPHA, yac.t[:, :], ALU.mult, ALU.add), [x2, yac], [yac])
        layernorm(yac, lng[3], lnb[3])
        P.dma('sp', y_o[r0:r0 + 128, :], yac.t[:, :], [yac], ['y_o'])
    P.release(f0)
    if _DBG:
        src = {'MIX': MIX, 'X1': X1, 'X2': X2}[_DBG]
        keys = [k for k in list(P.res_w.keys()) if isinstance(k, tuple) and isinstance(k[0], str) and k[0].startswith(_DBG)]
        P.dma('sp', dbg_o[:, :], src[:, :], keys, ['dbg_o'])
    P.emit(final_wait=['dbg_o', 'y_o', 'kvl_o', 'kr_o', 'memo0', 'memo1', 'Cp_o', 'np_o', 'mp_o', 'Cs_o', 'ns_o', 'ms_o'])
    return nc


_STOP = 99
_LAST = None


def kernel(**inputs):
    f32 = lambda a: np.ascontiguousarray(np.asarray(a), dtype=np.float32)
    x_prompt = f32(inputs["x_prompt"])
    x_sample = f32(inputs["x_sample"])
    nc = build_nc(stop_after=_STOP)
    if isinstance(nc, tuple):
        nc = nc[0]
    ropeinv = (1.0 / (10000.0 ** (np.arange(0, 32, 2, dtype=np.float32) / 32))).astype(np.float32)
    shared = {
        "ropeinv": ropeinv,
        "ln0_g": f32(inputs["ln0_g"]), "ln0_b": f32(inputs["ln0_b"]),
        "ln1_g": f32(inputs["ln1_g"])[0], "ln1_b": f32(inputs["ln1_b"])[0],
        "ln2_g": f32(inputs["ln2_g"])[0], "ln2_b": f32(inputs["ln2_b"])[0],
        "ln3_g": f32(inputs["ln3_g"])[0], "ln3_b": f32(inputs["ln3_b"])[0],
        "w_in": f32(inputs["w_in"])[0], "b_i": f32(inputs["b_i"])[0], "b_f": f32(inputs["b_f"])[0],
        "g_q": f32(inputs["g_q"])[0], "w_uq": f32(inputs["w_uq"])[0].reshape(384, 768),
        "g_kv": f32(inputs["g_kv"])[0], "w_uk": f32(inputs["w_uk"])[0].reshape(256, 512),
        "w_ukT": np.ascontiguousarray(f32(inputs["w_uk"])[0].transpose(2, 1, 0)).reshape(64, 2048),
        "w_uv": f32(inputs["w_uv"])[0].reshape(256, 512),
        "w_out": f32(inputs["w_out"])[0],
        "w_mq": f32(inputs["w_mq"])[0].reshape(D, D), "w_mk": f32(inputs["w_mk"])[0].reshape(D, D),
        "w_mv": f32(inputs["w_mv"])[0].reshape(D, D), "w_mo": f32(inputs["w_mo"])[0].reshape(D, D),
        "w_pq": f32(inputs["w_pq"])[0].reshape(D, D),
        "k1T": np.ascontiguousarray(f32(inputs["sub_k1"])[0].T), "k2T": np.ascontiguousarray(f32(inputs["sub_k2"])[0].T),
        "peer_u": f32(inputs["peer_u"])[0], "peer_v": f32(inputs["peer_v"])[0],
    }
    if _PH3:
        shared["ckv"] = f32(inputs["cache_kv_latent"])[0].reshape(NPOOL_ROWS, 256)
        shared["ckr"] = f32(inputs["cache_k_rope"])[0].reshape(NPOOL_ROWS, 32)
    page_table = np.ascontiguousarray(np.asarray(inputs["page_table"]), dtype=np.int32)
    stC = f32(inputs["state_C"])[0]
    stn = f32(inputs["state_n"])[0]
    stm = f32(inputs["state_m"])[0]
    cmk = f32(inputs["cache_mem_k"])[0].reshape(128, 256, 1024)
    cmv = f32(inputs["cache_mem_v"])[0].reshape(128, 256, 1024)
    memp = f32(inputs["mem_prompt"])
    in_maps = []
    for c in range(8):
        xin = np.zeros((NROW, D), np.float32)
        xin[:2048] = x_prompt[c]
        xin[2048:2048 + 64] = x_sample[16 * c:16 * c + 16].reshape(64, D)
        m = dict(shared)
        m.update({
            "xin": xin, "stC": np.ascontiguousarray(stC[16 * c:16 * c + 16]),
            "stn": np.ascontiguousarray(stn[16 * c:16 * c + 16]), "stm": np.ascontiguousarray(stm[16 * c:16 * c + 16]),
            "cmk": np.ascontiguousarray(cmk[16 * c:16 * c + 16]), "cmv": np.ascontiguousarray(cmv[16 * c:16 * c + 16]),
            "ptab": np.ascontiguousarray(page_table[16 * c:16 * c + 16].reshape(1024)),
            "memp": np.ascontiguousarray(memp[c]),
        })
        in_maps.append(m)
    res = run_bass_kernel_spmd(nc, in_maps, core_ids=list(range(8)))
    R = res.results
    global _LAST
    _LAST = R
    cat = lambda k, sl: np.stack([np.asarray(r[k])[sl] for r in R])
    y_prompt = cat("y", slice(0, 2048))
    y_sample = np.concatenate([np.asarray(r["y"])[2048:2112].reshape(16, 4, D) for r in R], 0)
    kvl_p = cat("kvl", slice(0, 2048))[None]
    kr_p = cat("kr", slice(0, 2048))[None]
    C_p = np.stack([np.asarray(r["Cp"]) for r in R])[None]
    n_p = np.stack([np.asarray(r["np_"]) for r in R])[None]
    m_p = np.stack([np.asarray(r["mp"]).reshape(4) for r in R])[None]
    mk_p = np.stack([np.asarray(r["mkp"]).reshape(256, 4, 256) for r in R])[None]
    mv_p = np.stack([np.asarray(r["mvp"]).reshape(256, 4, 256) for r in R])[None]
    kvl_s = np.concatenate([np.asarray(r["kvl"])[2048:2112].reshape(16, 4, 256) for r in R], 0)[None]
    kr_s = np.concatenate([np.asarray(r["kr"])[2048:2112].reshape(16, 4, 32) for r in R], 0)[None]
    C_s = np.concatenate([np.asarray(r["Cs"]) for r in R], 0)[None]
    n_s = np.concatenate([np.asarray(r["ns"]) for r in R], 0)[None]
    m_s = np.concatenate([np.asarray(r["ms"]) for r in R], 0)[None]
    outs = (y_prompt, y_sample, kvl_p, kr_p, C_p, n_p, m_p, mk_p, mv_p, kvl_s, kr_s, C_s, n_s, m_s)
    return tuple(np.ascontiguousarray(o, dtype=np.float32) for o in outs)
```
